# Optimizing a Trainium2 kernel written in Bass

```python
import math
import jax, jax.numpy as jnp
from jax import lax
import numpy as np

D_MODEL = 1024
BATCH = 8
SEQ = 4096
DEPTH = 4

CHUNK = 64
N_MIXERS = 3
EPS = 1e-6

GLA_HEADS = 4
GLA_DK = D_MODEL // 2
GLA_DV = D_MODEL
GLA_DKH = GLA_DK // GLA_HEADS
GLA_DVH = GLA_DV // GLA_HEADS
GLA_RANK = 16
GLA_TAU = 16.0
GLA_IN = 2 * GLA_DK + 2 * GLA_DV + GLA_RANK

CONV_WIDTH = 31

FOX_HEADS = 16
FOX_DH = D_MODEL // FOX_HEADS
FOX_IN = 4 * D_MODEL + FOX_HEADS
Q_BLOCK = 128

D_FF = 2816
N_EXPERTS = 8
TOP_K = 2
D_FF_EXPERT = 3584
MOE_BLOCK = 512

kernel_name = "hybrid_gla_conformer_fox_moe_adaln"


def rms_norm(x, g=None):
    xf = x.astype(jnp.float32)
    y = xf * lax.rsqrt(jnp.mean(xf * xf, axis=-1, keepdims=True) + EPS)
    if g is not None:
        y = y * g.astype(jnp.float32)
    return y.astype(x.dtype)


def gla_mixer(h, w_in, w_gate2, b_gate, gn_g, w_out):
    B, S, _ = h.shape
    nc = S // CHUNK
    f32 = jnp.float32
    q, k, v, r, glr = jnp.split(h @ w_in, [GLA_DK, 2 * GLA_DK, 2 * GLA_DK + GLA_DV,
                                          2 * GLA_DK + 2 * GLA_DV], axis=-1)
    log_a = jax.nn.log_sigmoid((glr @ w_gate2 + b_gate).astype(f32)) / GLA_TAU

    def to_chunks(t, dh):
        return jnp.moveaxis(t.reshape(B, nc, CHUNK, GLA_HEADS, dh), 1, 0)

    qc = to_chunks(q, GLA_DKH).astype(f32) * (GLA_DKH ** -0.5)
    kc = to_chunks(k, GLA_DKH).astype(f32)
    vc = to_chunks(v, GLA_DVH).astype(f32)
    cum = jnp.cumsum(to_chunks(log_a, GLA_DKH), axis=2)
    tot = cum[:, :, -1]
    kd = kc * jnp.exp(tot[:, :, None] - cum)

    def step(state, inp):
        q_i, kd_i, v_i, tot_i = inp
        state = state * jnp.exp(tot_i)[..., None] + jnp.einsum('bchk,bchv->bhkv', kd_i, v_i)
        o_i = jnp.einsum('bchk,bhkv->bchv', q_i, state)
        return state, o_i

    s0 = jnp.zeros((B, GLA_HEADS, GLA_DKH, GLA_DVH), f32)
    _, o = lax.scan(step, s0, (qc, kd, vc, tot))
    o = jnp.moveaxis(o, 0, 1).reshape(B, S, GLA_HEADS, GLA_DVH)
    o = rms_norm(o, gn_g.reshape(GLA_HEADS, GLA_DVH)).astype(h.dtype).reshape(B, S, GLA_DV)
    return (o * jax.nn.silu(r)) @ w_out


def conv_mixer(h, w_in, b_in, dw, dw_b, ln_g, ln_b, w_out, b_out):
    a, g = jnp.split(h @ w_in + b_in, 2, axis=-1)
    u = a * jax.nn.sigmoid(g)
    u = lax.conv_general_dilated(u, dw[:, None, :].astype(u.dtype), window_strides=(1,),
                                 padding=[(CONV_WIDTH - 1, 0)],
                                 dimension_numbers=('NWC', 'WIO', 'NWC'),
                                 feature_group_count=D_MODEL) + dw_b
    uf = u.astype(jnp.float32)
    mu = jnp.mean(uf, axis=-1, keepdims=True)
    var = jnp.mean(jnp.square(uf - mu), axis=-1, keepdims=True)
    u = ((uf - mu) * lax.rsqrt(var + EPS) * ln_g + ln_b).astype(h.dtype)
    return jax.nn.silu(u) @ w_out + b_out


def fox_mixer(h, w_in, b_f, qn_g, kn_g, w_out):
    B, S, D = h.shape
    f32 = jnp.float32
    q, k, v, fl, og = jnp.split(h @ w_in, [D, 2 * D, 3 * D, 3 * D + FOX_HEADS], axis=-1)
    q = rms_norm(q.reshape(B, S, FOX_HEADS, FOX_DH), qn_g)
    k = rms_norm(k.reshape(B, S, FOX_HEADS, FOX_DH), kn_g)
    v = v.reshape(B, S, FOX_HEADS, FOX_DH)
    cum = jnp.cumsum(jax.nn.log_sigmoid((fl + b_f).astype(f32)), axis=1)
    cum_k = jnp.transpose(cum, (0, 2, 1))
    nb = S // Q_BLOCK
    qb = jnp.moveaxis(q.reshape(B, nb, Q_BLOCK, FOX_HEADS, FOX_DH), 1, 0)
    cq = jnp.moveaxis(cum_k.reshape(B, FOX_HEADS, nb, Q_BLOCK), 2, 0)
    kpos = jnp.arange(S)
    scale = FOX_DH ** -0.5

    def block(args):
        q_i, c_i, i = args
        s = jnp.einsum('bqhd,bkhd->bhqk', q_i, k, preferred_element_type=f32) * scale
        s = s + (c_i[..., None] - cum_k[:, :, None, :])
        qpos = i * Q_BLOCK + jnp.arange(Q_BLOCK)
        s = jnp.where((kpos[None, :] <= qpos[:, None])[None, None], s, -jnp.inf)
        p = jax.nn.softmax(s, axis=-1)
        return jnp.einsum('bhqk,bkhd->bqhd', p.astype(v.dtype), v)

    o = lax.map(block, (qb, cq, jnp.arange(nb)))
    o = jnp.moveaxis(o, 0, 1).reshape(B, S, D)
    return (o * jax.nn.sigmoid(og)) @ w_out


def swiglu(h, w13, w2):
    a, b = jnp.split(h @ w13, 2, axis=-1)
    return (jax.nn.silu(a) * b) @ w2


def moe_swiglu(h, router, w13, w2):
    B, S, D = h.shape
    N = B * S
    xt = h.reshape(N, D)
    top_l, top_e = lax.top_k((xt @ router).astype(jnp.float32), TOP_K)
    gates = jax.nn.softmax(top_l, axis=-1)
    A = N * TOP_K
    e_flat = top_e.reshape(A)
    tok = jnp.arange(A, dtype=jnp.int32) // TOP_K
    order = jnp.argsort(e_flat)
    e_sorted = e_flat[order]
    counts = jnp.bincount(e_flat, length=N_EXPERTS)
    start = jnp.cumsum(counts) - counts
    padded = (counts + MOE_BLOCK - 1) // MOE_BLOCK * MOE_BLOCK
    pad_end = jnp.cumsum(padded)
    pad_start = pad_end - padded
    dest = pad_start[e_sorted] + jnp.arange(A, dtype=jnp.int32) - start[e_sorted]
    n_blocks = -(-A // MOE_BLOCK) + N_EXPERTS
    P = n_blocks * MOE_BLOCK
    slot_tok = jnp.zeros((P,), jnp.int32).at[dest].set(tok[order])
    slot_gate = jnp.zeros((P,), h.dtype).at[dest].set(gates.reshape(A)[order].astype(h.dtype))
    block_e = jnp.minimum(jnp.searchsorted(pad_end, jnp.arange(n_blocks, dtype=jnp.int32) * MOE_BLOCK,
                                           side='right'), N_EXPERTS - 1)

    def run_block(args):
        toks, g, e = args
        a, u = jnp.split(xt[toks] @ w13[e], 2, axis=-1)
        return ((jax.nn.silu(a) * u) @ w2[e]) * g[:, None]

    yb = lax.map(run_block, (slot_tok.reshape(n_blocks, MOE_BLOCK),
                             slot_gate.reshape(n_blocks, MOE_BLOCK), block_e))
    y = jnp.zeros_like(xt).at[slot_tok].add(yb.reshape(P, D))
    return y.reshape(B, S, D)


def setup_inputs(seed: int = 0) -> dict:
    key = jax.random.key(seed)
    keys = iter(jax.random.split(key, 48))
    D = D_MODEL
    n_gla = len(range(0, DEPTH, N_MIXERS))
    n_conv = len(range(1, DEPTH, N_MIXERS))
    n_fox = len(range(2, DEPTH, N_MIXERS))
    n_dense = len(range(0, DEPTH, 2))
    n_moe = len(range(1, DEPTH, 2))

    def nrm(shape, scale):
        return jax.random.normal(next(keys), shape, jnp.float32) * scale

    def gain(shape):
        return 1.0 + nrm(shape, 0.1)

    return {
        "x": nrm((BATCH, SEQ, D), 1.0),
        "c": nrm((BATCH, D), 1.0),
        "ada_w": nrm((DEPTH, D, 6 * D), 0.5 * D ** -0.5),
        "ada_b": nrm((DEPTH, 6 * D), 0.02),
        "gla_w_in": nrm((n_gla, D, GLA_IN), D ** -0.5),
        "gla_w_gate2": nrm((n_gla, GLA_RANK, GLA_DK), GLA_RANK ** -0.5),
        "gla_b_gate": 1.5 + nrm((n_gla, GLA_DK), 0.5),
        "gla_gn_g": gain((n_gla, GLA_DV)),
        "gla_w_out": nrm((n_gla, GLA_DV, D), GLA_DV ** -0.5),
        "conv_w_in": nrm((n_conv, D, 2 * D), D ** -0.5),
        "conv_b_in": nrm((n_conv, 2 * D), 0.02),
        "conv_dw": nrm((n_conv, CONV_WIDTH, D), CONV_WIDTH ** -0.5),
        "conv_dw_b": nrm((n_conv, D), 0.02),
        "conv_ln_g": gain((n_conv, D)),
        "conv_ln_b": nrm((n_conv, D), 0.02),
        "conv_w_out": nrm((n_conv, D, D), D ** -0.5),
        "conv_b_out": nrm((n_conv, D), 0.02),
        "fox_w_in": nrm((n_fox, D, FOX_IN), D ** -0.5),
        "fox_b_f": jax.random.uniform(next(keys), (n_fox, FOX_HEADS), jnp.float32, 1.0, 5.0),
        "fox_qn_g": gain((n_fox, FOX_DH)),
        "fox_kn_g": gain((n_fox, FOX_DH)),
        "fox_w_out": nrm((n_fox, D, D), D ** -0.5),
        "ffn_w13": nrm((n_dense, D, 2 * D_FF), D ** -0.5),
        "ffn_w2": nrm((n_dense, D_FF, D), D_FF ** -0.5),
        "moe_router": nrm((n_moe, D, N_EXPERTS), D ** -0.5),
        "moe_w13": nrm((n_moe, N_EXPERTS, D, 2 * D_FF_EXPERT), D ** -0.5),
        "moe_w2": nrm((n_moe, N_EXPERTS, D_FF_EXPERT, D), D_FF_EXPERT ** -0.5),
        "norm_f_g": gain((D,)),
    }


def reference(x, c, ada_w, ada_b,
              gla_w_in, gla_w_gate2, gla_b_gate, gla_gn_g, gla_w_out,
              conv_w_in, conv_b_in, conv_dw, conv_dw_b, conv_ln_g, conv_ln_b, conv_w_out, conv_b_out,
              fox_w_in, fox_b_f, fox_qn_g, fox_kn_g, fox_w_out,
              ffn_w13, ffn_w2, moe_router, moe_w13, moe_w2, norm_f_g):
    cond = jax.nn.silu(c)
    for i in range(DEPTH):
        mod = (cond @ ada_w[i] + ada_b[i])[:, None, :]
        sh1, sc1, g1, sh2, sc2, g2 = jnp.split(mod, 6, axis=-1)
        h = rms_norm(x) * (1 + sc1) + sh1
        m, j = i % N_MIXERS, i // N_MIXERS
        if m == 0:
            y = gla_mixer(h, gla_w_in[j], gla_w_gate2[j], gla_b_gate[j], gla_gn_g[j], gla_w_out[j])
        elif m == 1:
            y = conv_mixer(h, conv_w_in[j], conv_b_in[j], conv_dw[j], conv_dw_b[j],
                           conv_ln_g[j], conv_ln_b[j], conv_w_out[j], conv_b_out[j])
        else:
            y = fox_mixer(h, fox_w_in[j], fox_b_f[j], fox_qn_g[j], fox_kn_g[j], fox_w_out[j])
        x = x + g1 * y
        h = rms_norm(x) * (1 + sc2) + sh2
        if i % 2 == 0:
            y = swiglu(h, ffn_w13[i // 2], ffn_w2[i // 2])
        else:
            y = moe_swiglu(h, moe_router[i // 2], moe_w13[i // 2], moe_w2[i // 2])
        x = x + g2 * y
    return rms_norm(x, norm_f_g)
```

```python
import contextlib
import numpy as np
import concourse.bass as bass
import concourse.mybir as mybir
from concourse.bass_utils import run_bass_kernel_spmd

F32 = mybir.dt.float32
BF16 = mybir.dt.bfloat16
AF = mybir.ActivationFunctionType
ALU = mybir.AluOpType

D = 1024
EPS = 1e-6
WC = 2048
NCORES = 8

C_ID, C_TRIU, C_CIND, C_BONES, C_MASK, C_SEL = 0, 128, 256, 258, 386, 514
NCONST = 514 + 16 * 128


def make_consts():
    c = np.zeros((128, NCONST), np.float32)
    p = np.arange(128)
    c[:, C_ID:C_ID + 128] = np.eye(128, dtype=np.float32)
    c[:, C_TRIU:C_TRIU + 128] = ((p[:, None] > p[None, :]) & (p[:, None] // 64 == p[None, :] // 64))
    c[:, C_CIND + 0] = (p // 64 == 0)
    c[:, C_CIND + 1] = (p // 64 == 1)
    c[:, C_BONES:C_BONES + 128] = (p[:, None] // 64 == p[None, :] // 64)
    c[:, C_MASK:C_MASK + 128] = np.where(p[:, None] <= p[None, :], 0.0, -30000.0)
    for h in range(16):
        for r in (h, 32 + h, 64 + h):
            c[r, C_SEL + h * 128:C_SEL + (h + 1) * 128] = 1.0
    return c


class Sched:
    CE = ('pe', 'act', 'dve', 'pool')
    DQ = ('sp', 'pool', 'act')

    def __init__(self, nc, es, nd=8):
        self.nc = nc
        self.nd = nd
        self.eng = dict(pe=nc.tensor, act=nc.scalar, dve=nc.vector, pool=nc.gpsimd, sp=nc.sync)
        self.csem = {e: es.enter_context(nc.semaphore('c_' + e)) for e in self.CE}
        self.cnt = {e: 0 for e in self.CE}
        self.dsem = {q: [es.enter_context(nc.semaphore('d_%s%d' % (q, i))) for i in range(nd)] for q in self.DQ}
        self.dcnt = {q: 0 for q in self.DQ}
        self.ccsem = es.enter_context(nc.semaphore('ccs'))
        self.cccnt = 0
        self.waited = {e: {} for e in self.eng}
        self.drained = {e: 0 for e in self.CE}
        self.lw = {}
        self.rd = {}
        self.nins = 0

    def _wait(self, E, tok):
        sem, val, F, kind = tok
        w = self.waited[E]
        if w.get(sem.name, 0) >= val:
            return
        self.eng[E].wait_ge(sem, val)
        w[sem.name] = val

    def _deps(self, E, reads, writes):
        toks = []
        for r in reads:
            t = self.lw.get(r)
            if t is not None:
                toks.append((t, True))
        for wk in writes:
            t = self.lw.get(wk)
            if t is not None:
                toks.append((t, True))
            for t in self.rd.get(wk, {}).values():
                toks.append((t, False))
        for t, strong in toks:
            sem, val, F, kind = t
            if kind == 'c' and F == E:
                if E == 'pe' or not strong:
                    continue
                if self.drained[E] < val:
                    self.eng[E].drain()
                    self.drained[E] = self.cnt[E]
                continue
            self._wait(E, t)

    def _commit(self, tok, reads, writes):
        for r in reads:
            d = self.rd.setdefault(r, {})
            d[tok[0].name] = tok
        for wk in writes:
            self.lw[wk] = tok
            self.rd[wk] = {}

    def op(self, E, fn, reads=(), writes=()):
        self._deps(E, reads, writes)
        ins = fn(self.eng[E])
        self.cnt[E] += 1
        ins.then_inc(self.csem[E], 1)
        self._commit((self.csem[E], self.cnt[E], E, 'c'), reads, writes)
        self.nins += 1

    def dma(self, Q, out, in_, reads=(), writes=()):
        j = self.dcnt[Q]
        slot = j % self.nd
        sem = self.dsem[Q][slot]
        if j >= self.nd:
            self._wait(Q, (sem, 16 * (j // self.nd), Q, 'dma'))
        self._deps(Q, reads, writes)
        self.eng[Q].dma_start(out=out, in_=in_).then_inc(sem, 16)
        self.dcnt[Q] = j + 1
        self._commit((sem, 16 * (j // self.nd + 1), Q, 'dma'), reads, writes)

    def collective(self, in_ap, out_ap, reads=(), writes=()):
        self._deps('pool', reads, writes)
        self.nc.gpsimd.collective_compute(
            "AllGather", ALU.bypass, replica_groups=[list(range(NCORES))],
            ins=[in_ap], outs=[out_ap]).then_inc(self.ccsem)
        self.cccnt += 1
        self._commit((self.ccsem, self.cccnt, 'pool', 'cc'), reads, writes)

    def barrier(self):
        toks = [(self.csem[e], self.cnt[e], e, 'c') for e in self.CE if self.cnt[e] > 0]
        for q in self.DQ:
            j = self.dcnt[q]
            for slot in range(self.nd):
                n = (j - slot + self.nd - 1) // self.nd if j > slot else 0
                if n > 0:
                    toks.append((self.dsem[q][slot], 16 * n, q, 'dma'))
        for E in self.eng:
            for t in toks:
                if t[3] == 'c' and t[2] == E:
                    if self.drained[E] < t[1]:
                        self.eng[E].drain()
                        self.drained[E] = self.cnt[E]
                    continue
                self._wait(E, t)
        keep = {k: t for k, t in self.lw.items() if t[3] == 'cc'}
        self.lw.clear()
        self.rd.clear()
        self.lw.update(keep)


def pmaj(w):
    K, F = w.shape
    return np.ascontiguousarray(w.reshape(K // 128, 128, F).transpose(1, 0, 2)).reshape(128, -1)


def colform(v):
    return np.ascontiguousarray(np.asarray(v).reshape(-1, 128).T)


def ffn_units(F):
    nj = F // 128
    out = []
    j = 0
    while j < nj:
        n = min(4, nj - j)
        out.append((j, n))
        j += n
    return out


def pack_ffn(w13, w2, F):
    parts = []
    for (j0, n) in ffn_units(F):
        parts.append(pmaj(w13[:, j0 * 128:(j0 + n) * 128]))
        parts.append(pmaj(w13[:, F + j0 * 128:F + (j0 + n) * 128]))
        parts.append(pmaj(w2[j0 * 128:(j0 + n) * 128, :]))
    return np.concatenate(parts, axis=1)


def weight_plan(cfg):
    plan = []
    L = cfg['layers']
    DFF, DFFE, E = cfg['DFF'], cfg['DFFE'], cfg['E']
    cnt = {}
    for i, (mx, ff) in enumerate(L):
        g = i
        plan.append((g, 'ada_w%d' % i, 128, 8 * 6144, lambda I, i=i: pmaj(I['ada_w'][i])))
        plan.append((g, 'ada_bc%d' % i, 128, 48, lambda I, i=i: colform(I['ada_b'][i])))
        plan.append((g, 'ada_br%d' % i, 1, 6144, lambda I, i=i: np.asarray(I['ada_b'][i]).reshape(1, -1)))
        j = cnt.get(mx, 0)
        cnt[mx] = j + 1
        if mx == 'gla':
            plan.append((g, 'm_win%d' % i, 128, 8 * 3088, lambda I, j=j: pmaj(I['gla_w_in'][j])))
            plan.append((g, 'm_wg2%d' % i, 17, 512, lambda I, j=j: np.concatenate(
                [I['gla_w_gate2'][j], np.asarray(I['gla_b_gate'][j]).reshape(1, -1)], axis=0)))
            plan.append((g, 'm_gng%d' % i, 1, 1024, lambda I, j=j: np.asarray(I['gla_gn_g'][j]).reshape(1, -1)))
            plan.append((g, 'm_wout%d' % i, 128, 8 * 1024, lambda I, j=j: pmaj(I['gla_w_out'][j])))
        elif mx == 'conv':
            plan.append((g, 'm_win%d' % i, 128, 8 * 2048, lambda I, j=j: pmaj(I['conv_w_in'][j])))
            plan.append((g, 'm_bin%d' % i, 128, 16, lambda I, j=j: colform(I['conv_b_in'][j])))
            plan.append((g, 'm_dw%d' % i, 128, 8 * 31, lambda I, j=j: np.ascontiguousarray(
                np.asarray(I['conv_dw'][j]).T.reshape(8, 128, 31).transpose(1, 0, 2)).reshape(128, -1)))
            plan.append((g, 'm_dwb%d' % i, 1, 1024, lambda I, j=j: np.asarray(I['conv_dw_b'][j]).reshape(1, -1)))
            plan.append((g, 'm_lng%d' % i, 128, 8, lambda I, j=j: colform(I['conv_ln_g'][j])))
            plan.append((g, 'm_lnb%d' % i, 128, 8, lambda I, j=j: colform(I['conv_ln_b'][j])))
            plan.append((g, 'm_wout%d' % i, 128, 8 * 1024, lambda I, j=j: pmaj(I['conv_w_out'][j])))
            plan.append((g, 'm_bout%d' % i, 1, 1024, lambda I, j=j: np.asarray(I['conv_b_out'][j]).reshape(1, -1)))
        elif mx == 'fox':
            plan.append((g, 'm_win%d' % i, 128, 8 * 4112, lambda I, j=j: pmaj(I['fox_w_in'][j])))
            plan.append((g, 'm_bf%d' % i, 16, 1, lambda I, j=j: np.asarray(I['fox_b_f'][j]).reshape(16, 1)))
            plan.append((g, 'm_qg%d' % i, 128, 1, lambda I, j=j: np.tile(np.asarray(I['fox_qn_g'][j]), 2).reshape(128, 1)))
            plan.append((g, 'm_kg%d' % i, 128, 1, lambda I, j=j: np.tile(np.asarray(I['fox_kn_g'][j]), 2).reshape(128, 1)))
            plan.append((g, 'm_wout%d' % i, 128, 8 * 1024, lambda I, j=j: pmaj(I['fox_w_out'][j])))
        k = cnt.get(ff, 0)
        cnt[ff] = k + 1
        if ff == 'ffn':
            plan.append((g, 'f_w%d' % i, 128, 24 * DFF, lambda I, k=k: pack_ffn(I['ffn_w13'][k], I['ffn_w2'][k], DFF)))
        elif ff == 'moe':
            plan.append((g, 'f_rt%d' % i, 128, 8 * E, lambda I, k=k: pmaj(I['moe_router'][k])))
            for e in range(E):
                plan.append((g, 'f_w%d_%d' % (i, e), 128, 24 * DFFE,
                             lambda I, k=k, e=e: pack_ffn(I['moe_w13'][k][e], I['moe_w2'][k][e], DFFE)))
    g = len(L) - 1 if L else 0
    plan.append((g, 'nf_g', 1, 1024, lambda I: np.asarray(I['norm_f_g']).reshape(1, -1)))
    return plan


def plan_layout(cfg):
    plan = weight_plan(cfg)
    ngroups = max(len(cfg['layers']), 1)
    offs = [0] * ngroups
    layout = {}
    for (g, name, P, X, fn) in plan:
        layout[name] = (g, offs[g], P, X)
        offs[g] += (P * X + 63) // 64 * 64
    rows = []
    for g in range(ngroups):
        per = NCORES * WC
        rows.append(max((offs[g] + per - 1) // per, 1))
    return plan, layout, rows


def pack_weights(cfg, inputs):
    plan, layout, rows = plan_layout(cfg)
    bufs = [np.zeros((NCORES * r * WC,), np.float32) for r in rows]
    for (g, name, P, X, fn) in plan:
        a = np.asarray(fn(inputs), dtype=np.float32)
        assert a.shape == (P, X), (name, a.shape, P, X)
        off = layout[name][1]
        bufs[g][off:off + P * X] = a.reshape(-1)
    return [b.reshape(NCORES, r, WC) for b, r in zip(bufs, rows)]


class Prog:
    def __init__(self, cfg):
        self.cfg = cfg
        self.S = cfg['S']
        self.uid = 0

    def nm(self, base):
        self.uid += 1
        return '%s_%d' % (base, self.uid)

    def sb(self, es, base, shape, dt):
        return es.enter_context(self.nc.sbuf_tensor(self.nm(base), list(shape), dt, align_bytes=128))

    def bank(self, b):
        t = self.ps[b // 2]
        o = (b % 2) * 512
        return t, o

    def W(self, name):
        g, off, P, X = self.layout[name]
        flat = self.wflat[g]
        return flat[off:off + P * X].rearrange("(p x) -> p x", p=P)

    def wk(self, name):
        return ('wall', self.layout[name][0])

    def wload3(self, tile, name, writes):
        src = self.W(name).rearrange("p (c f) -> p c f", c=8)
        for c in range(8):
            self.sch.dma('pool', tile[:, c, :], src[:, c, :], reads=[self.wk(name)], writes=list(writes))

    def build(self):
        cfg = self.cfg
        S = self.S
        nc = bass.Bass("TRN2", target_bir_lowering=False)
        self.nc = nc
        self.plan, self.layout, self.rows = plan_layout(cfg)
        ng = len(self.rows)
        NSEQ = cfg.get('NSEQ', 1)
        COLL = cfg.get('COLL', True)
        self.x_all = nc.dram_tensor("x", [NSEQ * S, D], F32, kind="ExternalInput").ap()
        self.cvec_all = nc.dram_tensor("cvec", [128, 8 * NSEQ], F32, kind="ExternalInput").ap()
        self.consts = nc.dram_tensor("consts", [128, NCONST], F32, kind="ExternalInput").ap()
        self.y_all = nc.dram_tensor("y", [NSEQ * S, D], F32, kind="ExternalOutput").ap()
        if COLL:
            wsh = [nc.dram_tensor("wsh%d" % g, [self.rows[g], WC], F32, kind="ExternalInput") for g in range(ng)]
            bounce = [nc.dram_tensor("wb%d" % g, [self.rows[g], WC], F32) for g in range(ng)]
            wall = [nc.dram_tensor("wall%d" % g, [NCORES * self.rows[g], WC], F32) for g in range(ng)]
        else:
            wall = [nc.dram_tensor("wsh%d" % g, [NCORES * self.rows[g], WC], F32, kind="ExternalInput") for g in range(ng)]
        self.wflat = [w.ap().rearrange("a b -> (a b)") for w in wall]
        self.xs = nc.dram_tensor("xs", [S, D], F32).ap()
        self.modrow = nc.dram_tensor("modrow", [2 * max(len(cfg['layers']), 1), D], F32).ap()
        self.scr = {}

        with contextlib.ExitStack() as es:
            self.ges = es
            sch = Sched(nc, es)
            self.sch = sch
            self.ps = [es.enter_context(nc.psum_tensor("ps%d" % i, [128, 1024], F32)) for i in range(4)]
            self.ident = self.sb(es, "ident", [128, 128], F32)
            self.identb = self.sb(es, "identb", [128, 128], BF16)
            self.onesb = self.sb(es, "onesb", [128, 128], BF16)
            self.onec = self.sb(es, "onec", [128, 1], F32)
            self.epsc = self.sb(es, "epsc", [128, 1], F32)
            self.modc = self.sb(es, "modc", [128, max(len(cfg['layers']), 1) * 32], F32)
            sch.dma('sp', self.ident[:], self.consts[:, C_ID:C_ID + 128], writes=['ident'])
            sch.op('dve', lambda e: e.tensor_copy(out=self.identb[:], in_=self.ident[:]), reads=['ident'], writes=['identb'])
            sch.op('dve', lambda e: e.memset(self.onesb[:], 1.0), writes=['onesb'])
            sch.op('dve', lambda e: e.memset(self.onec[:], 1.0), writes=['onec'])
            sch.op('dve', lambda e: e.memset(self.epsc[:], EPS), writes=['epsc'])
            if COLL:
                for g in range(ng):
                    sch.dma('pool', bounce[g][:, :], wsh[g][:, :], writes=[('wb', g)])
                    sch.collective(bounce[g][:, :], wall[g][:, :], reads=[('wb', g)], writes=[('wall', g)])
            sch.barrier()
            for sq in range(NSEQ):
                self.x_in = self.x_all[sq * S:(sq + 1) * S, :]
                self.y = self.y_all[sq * S:(sq + 1) * S, :]
                self.cvec = self.cvec_all[:, sq * 8:(sq + 1) * 8]
                self.adaln_phase()
                first = True
                for i, (mx, ff) in enumerate(cfg['layers']):
                    src = self.x_in if first else self.xs
                    if mx == 'gla':
                        self.gla_phase(i, src)
                        first = False
                    elif mx == 'conv':
                        self.conv_phase(i, src)
                        first = False
                    elif mx == 'fox':
                        self.fox_phase(i, src)
                        first = False
                    src = self.x_in if first else self.xs
                    if ff in ('ffn', 'moe'):
                        self.ffn_phase(i, src, ff == 'moe')
                        first = False
                self.final_phase(self.x_in if first else self.xs)
            sch.barrier()
        return nc

    def rstd_ops(self, out_ap, in_ap, inv_n, key_in, key_out):
        sch = self.sch
        P = out_ap.shape[0]
        sch.op('act', lambda e: e.activation(out=out_ap, in_=in_ap, func=AF.Sqrt, scale=inv_n, bias=self.epsc[0:P, :]),
               reads=[key_in, 'epsc'], writes=[key_out])
        sch.op('dve', lambda e: e.reciprocal(out=out_ap, in_=out_ap), reads=[key_out], writes=[key_out])

    def prenorm_block(self, T, src, tok0, ntile, li, which, hT, hT_key, hT32=None):
        sch = self.sch
        sc = self.modc[:, li * 32 + which * 16 + 8: li * 32 + which * 16 + 16]
        sh = self.modc[:, li * 32 + which * 16: li * 32 + which * 16 + 8]
        xn = T['xn']
        for t in range(ntile):
            xt = T['xt'][t % len(T['xt'])]
            kx = ('xt', t % len(T['xt']))
            sch.dma('sp', xt[:], src[tok0 + t * 128: tok0 + (t + 1) * 128, :],
                    reads=[('xs', (tok0 // 128) + t)], writes=[kx])
            ssq = T['ssq']
            sch.op('act', lambda e, xt=xt, t=t: e.activation(out=T['junk'][:], in_=xt[:], func=AF.Square,
                                                             accum_out=ssq[:, t:t + 1]),
                   reads=[kx], writes=['junk', ('ssq', t)])
            self.rstd_ops(ssq[:, t:t + 1], ssq[:, t:t + 1], 1.0 / D, ('ssq', t), ('ssq', t))
            sch.op('dve', lambda e, xt=xt, t=t: e.tensor_scalar(out=xn[:, t, :], in0=xt[:], scalar1=ssq[:, t:t + 1],
                                                                scalar2=None, op0=ALU.mult),
                   reads=[kx, ('ssq', t)], writes=[('xn', t)])
        nb = T['tbanks']
        for c in range(8):
            b = nb[c % len(nb)]
            pt, po = self.bank(b)

            def tr(e, c=c, pt=pt, po=po):
                ins = None
                for t in range(ntile):
                    ins = e.transpose(out=pt[:, po + t * 128: po + (t + 1) * 128],
                                      in_=xn[:, t, c * 128:(c + 1) * 128], identity=self.ident[:])
                return ins
            sch.op('pe', tr, reads=[('xn', t) for t in range(ntile)] + ['ident'], writes=[('ps', b)])
            if hT32 is not None:
                sch.op('act', lambda e, c=c, pt=pt, po=po: e.activation(
                    out=hT32[:, c, 0:ntile * 128], in_=pt[:, po:po + ntile * 128], func=AF.Identity,
                    scale=sc[:, c:c + 1], bias=sh[:, c:c + 1]),
                    reads=[('ps', b), 'modc'], writes=[('hT32', c)])
                sch.op('pool', lambda e, c=c: e.tensor_copy(out=hT[:, c, 0:ntile * 128], in_=hT32[:, c, 0:ntile * 128]),
                       reads=[('hT32', c)], writes=[(hT_key, c)])
            else:
                sch.op('act', lambda e, c=c, pt=pt, po=po: e.activation(
                    out=hT[:, c, 0:ntile * 128], in_=pt[:, po:po + ntile * 128], func=AF.Identity,
                    scale=sc[:, c:c + 1], bias=sh[:, c:c + 1]),
                    reads=[('ps', b), 'modc'], writes=[(hT_key, c)])

    def prenorm_tiles(self, es, nxt=2):
        T = {}
        T['xt'] = [self.sb(es, "xt", [128, D], F32) for _ in range(nxt)]
        T['xn'] = self.sb(es, "xn", [128, 4, D], F32)
        T['junk'] = self.sb(es, "junk", [128, D], F32)
        T['ssq'] = self.sb(es, "ssq", [128, 4], F32)
        T['tbanks'] = [6, 7]
        return T

    def load_grow(self, es, li, which):
        gB = self.sb(es, "gB", [128, D], F32)
        r = li * 2 + which
        self.sch.dma('sp', gB[:], self.modrow[r:r + 1, :].to_broadcast([128, D]), reads=['modrow'], writes=['gB'])
        return gB

    def residual_store(self, R, ykey, ypt, tok0, src):
        sch = self.sch
        i = R['i'] % len(R['xr'])
        R['i'] += 1
        xr = R['xr'][i]
        tmp = R['tmp'][i]
        kt = ('xs', tok0 // 128)
        sch.dma('sp', xr[:], src[tok0:tok0 + 128, :], reads=[kt], writes=[('xr', i)])
        sch.op('dve', lambda e: e.tensor_tensor(out=tmp[:], in0=ypt, in1=R['gB'][:], op=ALU.mult),
               reads=list(ykey) + ['gB'], writes=[('rtmp', i)])
        sch.op('pool', lambda e: e.tensor_tensor(out=tmp[:], in0=tmp[:], in1=xr[:], op=ALU.add),
               reads=[('rtmp', i), ('xr', i)], writes=[('rtmp', i)])
        sch.dma('sp', self.xs[tok0:tok0 + 128, :], tmp[:], reads=[('rtmp', i)], writes=[kt])

    def residual_tiles(self, es, gB):
        return dict(i=0, gB=gB, xr=[self.sb(es, "xr", [128, D], F32) for _ in range(2)],
                    tmp=[self.sb(es, "rtmp", [128, D], F32) for _ in range(2)])

    def out_proj(self, OT, otkey, wout, ntile, tok0, R, src, bias_row=None):
        sch = self.sch
        pt = self.ps[2]
        for t in range(ntile):
            def mm(e, t=t):
                ins = None
                for n in range(2):
                    for c in range(8):
                        ins = e.matmul(pt[:, n * 512:(n + 1) * 512], OT[:, c, t * 128:(t + 1) * 128],
                                       wout[:, c, n * 512:(n + 1) * 512], start=(c == 0),
                                       stop=(c == 7 and bias_row is None))
                    if bias_row is not None:
                        ins = e.matmul(pt[:, n * 512:(n + 1) * 512], self.onesb[0:1, 0:128],
                                       bias_row[0:1, n * 512:(n + 1) * 512], start=False, stop=True)
                return ins
            sch.op('pe', mm, reads=[(otkey, c) for c in range(8)] + ['wout', 'brow', 'onesb'],
                   writes=[('ps', 4), ('ps', 5)])
            self.residual_store(R, [('ps', 4), ('ps', 5)], pt[:, :], tok0 + t * 128, src)

    def adaln_phase(self):
        sch = self.sch
        nc = self.nc
        L = self.cfg['layers']
        with contextlib.ExitStack() as es:
            cc = self.sb(es, "cc", [128, 8], F32)
            cb = self.sb(es, "cb", [128, 8], BF16)
            wA = [self.sb(es, "wA", [128, 8, 1024], BF16) for _ in range(2)]
            bc = self.sb(es, "bc", [128, 48], F32)
            br = self.sb(es, "br", [1, 6144], F32)
            row = self.sb(es, "row", [1, 1024], F32)
            sch.dma('sp', cc[:], self.cvec[:, :], writes=['cc'])
            sch.op('act', lambda e: e.activation(out=cc[:], in_=cc[:], func=AF.Silu), reads=['cc'], writes=['cc'])
            sch.op('dve', lambda e: e.tensor_copy(out=cb[:], in_=cc[:]), reads=['cc'], writes=['cb'])
            k = 0
            for i in range(len(L)):
                sch.dma('sp', bc[:], self.W('ada_bc%d' % i), reads=[self.wk('ada_bc%d' % i)], writes=['bc'])
                sch.dma('sp', br[:], self.W('ada_br%d' % i), reads=[self.wk('ada_br%d' % i)], writes=['br'])
                aw = self.W('ada_w%d' % i).rearrange("p (c f) -> p c f", c=8)
                for g in range(6):
                    w = wA[k % 2]
                    kw = ('wA', k % 2)
                    k += 1
                    for c0 in (0, 4):
                        sch.dma('pool', w[:, c0:c0 + 4, :], aw[:, c0:c0 + 4, g * 1024:(g + 1) * 1024], reads=[self.wk('ada_w%d' % i)], writes=[kw])
                    pt, po = self.bank(0)
                    if g in (0, 1, 3, 4):
                        def mm(e, w=w, pt=pt, po=po):
                            ins = None
                            for oc in range(8):
                                for c in range(8):
                                    ins = e.matmul(pt[:, po + oc:po + oc + 1], w[:, c, oc * 128:(oc + 1) * 128],
                                                   cb[:, c:c + 1], start=(c == 0), stop=(c == 7))
                            return ins
                        sch.op('pe', mm, reads=[kw, 'cb'], writes=[('ps', 0)])
                        which = 0 if g < 2 else 1
                        isscale = g in (1, 4)
                        dst = self.modc[:, i * 32 + which * 16 + (8 if isscale else 0): i * 32 + which * 16 + (16 if isscale else 8)]
                        sch.op('dve', lambda e, dst=dst, pt=pt, po=po, g=g: e.tensor_tensor(
                            out=dst, in0=pt[:, po:po + 8], in1=bc[:, g * 8:(g + 1) * 8], op=ALU.add),
                            reads=[('ps', 0), 'bc'], writes=['modc'])
                        if isscale:
                            sch.op('dve', lambda e, dst=dst: e.tensor_scalar(out=dst, in0=dst, scalar1=1.0, scalar2=None,
                                                                             op0=ALU.add), reads=['modc'], writes=['modc'])
                    else:
                        which = 0 if g == 2 else 1
                        for n in range(2):
                            def mm(e, w=w, pt=pt, po=po, n=n):
                                ins = None
                                for c in range(8):
                                    ins = e.matmul(pt[0:1, po:po + 512], cb[:, c:c + 1], w[:, c, n * 512:(n + 1) * 512],
                                                   start=(c == 0), stop=(c == 7))
                                return ins
                            sch.op('pe', mm, reads=[kw, 'cb'], writes=[('ps', 0)])
                            sch.op('dve', lambda e, pt=pt, po=po, n=n, g=g: e.tensor_tensor(
                                out=row[0:1, n * 512:(n + 1) * 512], in0=pt[0:1, po:po + 512],
                                in1=br[0:1, g * 1024 + n * 512: g * 1024 + (n + 1) * 512], op=ALU.add),
                                reads=[('ps', 0), 'br'], writes=['row'])
                        r = i * 2 + which
                        sch.dma('sp', self.modrow[r:r + 1, :], row[0:1, :], reads=['row'], writes=['modrow'])
            sch.barrier()

    def ffn_phase(self, li, src, moe):
        sch = self.sch
        cfg = self.cfg
        S = self.S
        F = cfg['DFFE'] if moe else cfg['DFF']
        E = cfg['E'] if moe else 1
        units = ffn_units(F)
        TB = min(1024, S)
        NT = TB // 128
        NQ = TB // 512
        with contextlib.ExitStack() as es:
            T = self.prenorm_tiles(es)
            hT = self.sb(es, "hT", [128, 8, TB], BF16)
            hT32 = self.sb(es, "hT32", [128, 8, 512], F32) if moe else None
            acc = self.sb(es, "acc", [128, NT, D], F32)
            act = [self.sb(es, "act", [128, 4, TB], BF16) for _ in range(2)]
            sa = [self.sb(es, "sa", [128, 512], F32) for _ in range(2)]
            wu = [self.sb(es, "wu", [128, 12288], BF16) for _ in range(2)]
            gB = self.load_grow(es, li, 1)
            R = self.residual_tiles(es, gB)
            if moe:
                rt = self.sb(es, "rt", [128, 8, E], F32)
                G = self.sb(es, "G", [128, NT, E], F32)
                lg = self.sb(es, "lg", [128, E], F32)
                ex = self.sb(es, "ex", [128, E], F32)
                mx8 = self.sb(es, "mx8", [128, 8], F32)
                lg8 = self.sb(es, "lg8", [128, 8], F32)
                sm = self.sb(es, "sm", [128, 4], F32)
                sch.dma('sp', rt[:], self.W('f_rt%d' % li).rearrange("p (c e) -> p c e", c=8), reads=[self.wk('f_rt%d' % li)], writes=['rt'])
                if E < 8:
                    sch.op('dve', lambda e: e.memset(lg8[:], -1e30), writes=['lg8'])
            ui = 0
            si = 0
            for blk in range(S // TB):
                tok0 = blk * TB
                for q in range(NQ):
                    if moe:
                        hsub = self.sb(es, "hsub", [128, 8, 512], BF16) if False else None
                    hview = hT[:, :, q * 512:(q + 1) * 512]
                    self.prenorm_block(T, src, tok0 + q * 512, 4, li, 1, hview, ('hT', q), hT32=hT32)
                    if moe:
                        for t in range(4):
                            tt = q * 4 + t
                            pt, po = self.bank(6 + (t % 2))

                            def mm(e, pt=pt, po=po, t=t):
                                ins = None
                                for c in range(8):
                                    ins = e.matmul(pt[:, po:po + E], hT32[:, c, t * 128:(t + 1) * 128], rt[:, c, :],
                                                   start=(c == 0), stop=(c == 7))
                                return ins
                            kb = ('ps', 6 + (t % 2))
                            sch.op('pe', mm, reads=[('hT32', c) for c in range(8)] + ['rt'], writes=[kb])
                            sch.op('dve', lambda e, pt=pt, po=po: e.tensor_copy(out=lg8[:, 0:E], in_=pt[:, po:po + E]),
                                   reads=[kb], writes=['lg8'])
                            sch.op('dve', lambda e: e.max(out=mx8[:], in_=lg8[:]), reads=['lg8'], writes=['mx8'])
                            sch.op('dve', lambda e: e.tensor_scalar(out=sm[:, 0:1], in0=mx8[:, 0:1], scalar1=-1.0, scalar2=None,
                                                                    op0=ALU.mult), reads=['mx8'], writes=['sm'])
                            sch.op('act', lambda e: e.activation(out=ex[:], in_=lg8[:, 0:E], func=AF.Exp, bias=sm[:, 0:1], scale=1.0),
                                   reads=['lg8', 'sm'], writes=['ex'])
                            sch.op('dve', lambda e: e.scalar_tensor_tensor(out=ex[:], in0=lg8[:, 0:E], scalar=mx8[:, 1:2], in1=ex[:],
                                                                           op0=ALU.is_ge, op1=ALU.mult),
                                   reads=['lg8', 'mx8', 'ex'], writes=['ex'])
                            sch.op('dve', lambda e: e.reduce_sum(out=sm[:, 1:2], in_=ex[:], axis=mybir.AxisListType.X),
                                   reads=['ex'], writes=['sm'])
                            sch.op('dve', lambda e: e.reciprocal(out=sm[:, 2:3], in_=sm[:, 1:2]), reads=['sm'], writes=['sm'])
                            sch.op('dve', lambda e, tt=tt: e.tensor_scalar(out=G[:, tt, :], in0=ex[:], scalar1=sm[:, 2:3],
                                                                          scalar2=None, op0=ALU.mult),
                                   reads=['ex', 'sm'], writes=[('G', tt)])
                first = True
                for ex_i in range(E):
                    wname = ('f_w%d_%d' % (li, ex_i)) if moe else ('f_w%d' % li)
                    wv = self.W(wname)
                    for (j0, n) in units:
                        w = wu[ui % 2]
                        kw = ('wu', ui % 2)
                        a_t = act[ui % 2]
                        ka = ('act', ui % 2)
                        ui += 1
                        for pc in range(n):
                            sch.dma('pool', w[:, pc * 3072:(pc + 1) * 3072], wv[:, j0 * 3072 + pc * 3072: j0 * 3072 + (pc + 1) * 3072],
                                    reads=[self.wk(wname)], writes=[kw])
                        for jj in range(n):
                            for q in range(NQ):
                                ba, bb = (0, 1) if (si % 2 == 0) else (2, 3)
                                s_t = sa[si % 2]
                                ks = ('sa', si % 2)
                                si += 1
                                pa, oa = self.bank(ba)
                                pb, ob = self.bank(bb)

                                def mm(e, w=w, jj=jj, q=q, n=n, pa=pa, oa=oa, pb=pb, ob=ob):
                                    ins = None
                                    for c in range(8):
                                        ins = e.matmul(pa[:, oa:oa + 512], w[:, c * n * 128 + jj * 128: c * n * 128 + (jj + 1) * 128],
                                                       hT[:, c, q * 512:(q + 1) * 512], start=(c == 0), stop=(c == 7))
                                    for c in range(8):
                                        o = 8 * n * 128
                                        ins = e.matmul(pb[:, ob:ob + 512], w[:, o + c * n * 128 + jj * 128: o + c * n * 128 + (jj + 1) * 128],
                                                       hT[:, c, q * 512:(q + 1) * 512], start=(c == 0), stop=(c == 7))
                                    return ins
                                sch.op('pe', mm, reads=[kw] + [('hT', q)] + [(('hT', q), c) for c in range(8)],
                                       writes=[('ps', ba), ('ps', bb)])
                                sch.op('act', lambda e, s_t=s_t, pa=pa, oa=oa: e.activation(out=s_t[:], in_=pa[:, oa:oa + 512], func=AF.Silu),
                                       reads=[('ps', ba)], writes=[ks])
                                sch.op('dve', lambda e, s_t=s_t, pb=pb, ob=ob, a_t=a_t, jj=jj, q=q: e.tensor_tensor(
                                    out=a_t[:, jj, q * 512:(q + 1) * 512], in0=s_t[:], in1=pb[:, ob:ob + 512], op=ALU.mult),
                                    reads=[ks, ('ps', bb)], writes=[ka])
                        for t in range(NT):
                            pt = self.ps[2]

                            def mm2(e, w=w, n=n, t=t, a_t=a_t, pt=pt):
                                ins = None
                                o = 16 * n * 128
                                for nn in range(2):
                                    for jj in range(n):
                                        ins = e.matmul(pt[:, nn * 512:(nn + 1) * 512], a_t[:, jj, t * 128:(t + 1) * 128],
                                                       w[:, o + jj * 1024 + nn * 512: o + jj * 1024 + (nn + 1) * 512],
                                                       start=(jj == 0), stop=(jj == n - 1))
                                return ins
                            sch.op('pe', mm2, reads=[kw, ka], writes=[('ps', 4), ('ps', 5)])
                            if moe:
                                if first:
                                    sch.op('dve', lambda e, t=t, pt=pt, ex_i=ex_i: e.tensor_scalar(
                                        out=acc[:, t, :], in0=pt[:, :], scalar1=G[:, t, ex_i:ex_i + 1], scalar2=None, op0=ALU.mult),
                                        reads=[('ps', 4), ('ps', 5), ('G', t)], writes=[('acc', t)])
                                else:
                                    sch.op('dve', lambda e, t=t, pt=pt, ex_i=ex_i: e.scalar_tensor_tensor(
                                        out=acc[:, t, :], in0=pt[:, :], scalar=G[:, t, ex_i:ex_i + 1], in1=acc[:, t, :],
                                        op0=ALU.mult, op1=ALU.add),
                                        reads=[('ps', 4), ('ps', 5), ('G', t), ('acc', t)], writes=[('acc', t)])
                            else:
                                if first:
                                    sch.op('act', lambda e, t=t, pt=pt: e.activation(out=acc[:, t, :], in_=pt[:, :], func=AF.Identity),
                                           reads=[('ps', 4), ('ps', 5)], writes=[('acc', t)])
                                else:
                                    sch.op('dve', lambda e, t=t, pt=pt: e.tensor_tensor(out=acc[:, t, :], in0=pt[:, :], in1=acc[:, t, :],
                                                                                       op=ALU.add),
                                           reads=[('ps', 4), ('ps', 5), ('acc', t)], writes=[('acc', t)])
                        first = False
                for t in range(NT):
                    self.residual_store(R, [('acc', t)], acc[:, t, :], tok0 + t * 128, src)
            sch.barrier()

    def final_phase(self, src):
        sch = self.sch
        S = self.S
        with contextlib.ExitStack() as es:
            gB = self.sb(es, "nfg", [128, D], F32)
            sch.dma('sp', gB[:], self.W('nf_g').to_broadcast([128, D]), reads=[self.wk('nf_g')], writes=['nfg'])
            xt = [self.sb(es, "fxt", [128, D], F32) for _ in range(3)]
            junk = self.sb(es, "fjunk", [128, D], F32)
            ssq = self.sb(es, "fssq", [128, 4], F32)
            for tt in range(S // 128):
                i = tt % 3
                x = xt[i]
                kx = ('fxt', i)
                ks = ('fssq', tt % 4)
                sc = ssq[:, tt % 4: tt % 4 + 1]
                sch.dma('sp', x[:], src[tt * 128:(tt + 1) * 128, :], reads=[('xs', tt)], writes=[kx])
                sch.op('act', lambda e, x=x, sc=sc: e.activation(out=junk[:], in_=x[:], func=AF.Square, accum_out=sc),
                       reads=[kx], writes=['fjunk', ks])
                self.rstd_ops(sc, sc, 1.0 / D, ks, ks)
                sch.op('dve', lambda e, x=x, sc=sc: e.scalar_tensor_tensor(out=x[:], in0=x[:], scalar=sc, in1=gB[:],
                                                                          op0=ALU.mult, op1=ALU.mult),
                       reads=[kx, ks, 'nfg'], writes=[kx])
                sch.dma('sp', self.y[tt * 128:(tt + 1) * 128, :], x[:], reads=[kx], writes=[('y', tt)])
            sch.barrier()

    def dscr(self, base, shape, dt=BF16):
        if base not in self.scr:
            self.scr[base] = self.nc.dram_tensor(self.nm(base), list(shape), dt).ap()
        return self.scr[base]

    def out_proj2(self, OT, otkeys, wout, ntile, tok0, R, src, bias_row=None):
        sch = self.sch
        pt = self.ps[2]
        for t in range(ntile):
            def mm(e, t=t):
                ins = None
                for n in range(2):
                    for c in range(8):
                        ins = e.matmul(pt[:, n * 512:(n + 1) * 512], OT[:, c, t * 128:(t + 1) * 128],
                                       wout[:, c, n * 512:(n + 1) * 512], start=(c == 0),
                                       stop=(c == 7 and bias_row is None))
                    if bias_row is not None:
                        ins = e.matmul(pt[:, n * 512:(n + 1) * 512], self.onesb[0:1, 0:128],
                                       bias_row[0:1, n * 512:(n + 1) * 512], start=False, stop=True)
                return ins
            sch.op('pe', mm, reads=list(otkeys) + ['wout', 'brow', 'onesb'], writes=[('ps', 4), ('ps', 5)])
            self.residual_store(R, [('ps', 4), ('ps', 5)], pt[:, :], tok0 + t * 128, src)

    def gla_phase(self, li, src):
        sch = self.sch
        S = self.S
        NB = S // 512
        NCHK = S // 64
        qTs = self.dscr("qTs%d" % li, [4, 128, S])
        kds = self.dscr("kds%d" % li, [S, 512])
        vs = self.dscr("vs%d" % li, [S, 1024])
        srgs = self.dscr("srgs%d" % li, [S, 1024])
        wn = 'm_win%d' % li
        with contextlib.ExitStack() as esAB:
            decay = self.sb(esAB, "decay", [128, 4, NCHK], F32)
            with contextlib.ExitStack() as es:
                T = self.prenorm_tiles(es)
                hT = self.sb(es, "hT", [128, 8, 512], BF16)
                win = self.sb(es, "win", [128, 8, 3088], BF16)
                wg2 = self.sb(es, "wg2", [17, 512], F32)
                gnB = self.sb(es, "gnB", [128, D], F32)
                triu = self.sb(es, "triu", [128, 128], F32)
                cind = self.sb(es, "cind", [128, 2], F32)
                glr = self.sb(es, "glr", [17, 512], F32)
                e1 = [self.sb(es, "e1", [128, 512], F32) for _ in range(2)]
                nla = [self.sb(es, "nla", [128, 512], F32) for _ in range(2)]
                dec = [self.sb(es, "dec", [128, 512], F32) for _ in range(2)]
                qsb = [self.sb(es, "qsb", [128, 512], BF16) for _ in range(2)]
                kdsb = [self.sb(es, "kdsb", [128, 512], BF16) for _ in range(2)]
                vsb = [self.sb(es, "vsb", [128, D], BF16) for _ in range(2)]
                srsb = [self.sb(es, "srsb", [128, D], F32) for _ in range(2)]
                srgsb = [self.sb(es, "srgsb", [128, D], BF16) for _ in range(2)]
                self.wload3(win, wn, ['win'])
                sch.dma('sp', wg2[:], self.W('m_wg2%d' % li), reads=[self.wk(wn)], writes=['wg2'])
                sch.dma('sp', gnB[:], self.W('m_gng%d' % li).to_broadcast([128, D]), reads=[self.wk(wn)], writes=['gnB'])
                sch.dma('sp', triu[:], self.consts[:, C_TRIU:C_TRIU + 128], writes=['triu'])
                sch.dma('sp', cind[:], self.consts[:, C_CIND:C_CIND + 2], writes=['cind'])
                sch.op('dve', lambda e: e.memset(glr[:], 1.0), writes=['glr'])
                ri = 0
                for blk in range(NB):
                    self.prenorm_block(T, src, blk * 512, 4, li, 0, hT, 'hT')
                    hk = [('hT', c) for c in range(8)]
                    for hd in range(4):
                        b = hd % 2
                        pt, po = self.bank(b)

                        def mm(e, hd=hd, pt=pt, po=po):
                            ins = None
                            for c in range(8):
                                ins = e.matmul(pt[:, po:po + 512], win[:, c, hd * 128:(hd + 1) * 128], hT[:, c, :],
                                               start=(c == 0), stop=(c == 7))
                            return ins
                        sch.op('pe', mm, reads=hk + ['win'], writes=[('ps', b)])
                        q = qsb[hd % 2]
                        sch.op('act', lambda e, q=q, pt=pt, po=po: e.activation(out=q[:], in_=pt[:, po:po + 512], func=AF.Identity,
                                                                               scale=float(128 ** -0.5)),
                               reads=[('ps', b)], writes=[('qsb', hd % 2)])
                        sch.dma('sp', qTs[hd, :, blk * 512:(blk + 1) * 512], q[:], reads=[('qsb', hd % 2)], writes=[('qTs', blk)])
                    pt, po = self.bank(2)

                    def mmg(e, pt=pt, po=po):
                        ins = None
                        for c in range(8):
                            ins = e.matmul(pt[0:16, po:po + 512], win[:, c, 3072:3088], hT[:, c, :], start=(c == 0), stop=(c == 7))
                        return ins
                    sch.op('pe', mmg, reads=hk + ['win'], writes=[('ps', 2)])
                    sch.op('act', lambda e, pt=pt, po=po: e.activation(out=glr[0:16, :], in_=pt[0:16, po:po + 512], func=AF.Identity),
                           reads=[('ps', 2)], writes=['glr'])
                    for t in range(4):
                        tt = blk * 4 + t
                        i2 = ri % 2
                        ri += 1
                        p3, o3 = self.bank(3)
                        sch.op('pe', lambda e, t=t, p3=p3, o3=o3: e.matmul(p3[:, o3:o3 + 512], glr[0:17, t * 128:(t + 1) * 128], wg2[0:17, :],
                                                                        start=True, stop=True),
                               reads=['glr', 'wg2'], writes=[('ps', 3)])
                        sch.op('act', lambda e, i2=i2, p3=p3, o3=o3: e.activation(out=e1[i2][:], in_=p3[:, o3:o3 + 512], func=AF.Exp, scale=-1.0),
                               reads=[('ps', 3)], writes=[('e1', i2)])
                        sch.op('act', lambda e, i2=i2: e.activation(out=nla[i2][:], in_=e1[i2][:], func=AF.Ln, bias=self.onec[:, 0:1], scale=1.0),
                               reads=[('e1', i2), 'onec'], writes=[('nla', i2)])
                        p0, o0 = self.bank(0)
                        sch.op('pe', lambda e, i2=i2, p0=p0, o0=o0: e.matmul(p0[:, o0:o0 + 512], triu[:, :], nla[i2][:], start=True, stop=True),
                               reads=['triu', ('nla', i2)], writes=[('ps', 0)])
                        sch.op('act', lambda e, i2=i2, p0=p0, o0=o0: e.activation(out=dec[i2][:], in_=p0[:, o0:o0 + 512], func=AF.Exp, scale=-1.0 / 16.0),
                               reads=[('ps', 0)], writes=[('dec', i2)])
                        p1, o1 = self.bank(1)

                        def mmk(e, t=t, p1=p1, o1=o1):
                            ins = None
                            for c in range(8):
                                ins = e.matmul(p1[:, o1:o1 + 512], hT[:, c, t * 128:(t + 1) * 128], win[:, c, 512:1024],
                                               start=(c == 0), stop=(c == 7))
                            return ins
                        sch.op('pe', mmk, reads=hk + ['win'], writes=[('ps', 1)])
                        sch.op('dve', lambda e, i2=i2, p1=p1, o1=o1: e.tensor_tensor(out=kdsb[i2][:], in0=p1[:, o1:o1 + 512], in1=dec[i2][:], op=ALU.mult),
                               reads=[('ps', 1), ('dec', i2)], writes=[('kdsb', i2)])
                        sch.dma('sp', kds[tt * 128:(tt + 1) * 128, :], kdsb[i2][:], reads=[('kdsb', i2)], writes=[('kds', tt)])
                        p2, o2 = self.bank(2)

                        def mmt(e, i2=i2, p2=p2, o2=o2):
                            ins = None
                            for hd in range(4):
                                ins = e.matmul(p2[:, o2 + hd * 2:o2 + hd * 2 + 2], nla[i2][:, hd * 128:(hd + 1) * 128], cind[:, 0:2],
                                               start=True, stop=True)
                            return ins
                        sch.op('pe', mmt, reads=[('nla', i2), 'cind'], writes=[('ps', 2)])

                        def dcy(e, tt=tt, p2=p2, o2=o2):
                            ins = None
                            for hd in range(4):
                                ins = e.activation(out=decay[:, hd, tt * 2:tt * 2 + 2], in_=p2[:, o2 + hd * 2:o2 + hd * 2 + 2],
                                                   func=AF.Exp, scale=-1.0 / 16.0)
                            return ins
                        sch.op('act', dcy, reads=[('ps', 2)], writes=['decay'])
                        pv = self.ps[2]

                        def mmv(e, t=t, base=1024, pv=pv):
                            ins = None
                            for n in range(2):
                                for c in range(8):
                                    ins = e.matmul(pv[:, n * 512:(n + 1) * 512], hT[:, c, t * 128:(t + 1) * 128],
                                                   win[:, c, base + n * 512: base + (n + 1) * 512], start=(c == 0), stop=(c == 7))
                            return ins
                        sch.op('pe', mmv, reads=hk + ['win'], writes=[('ps', 4), ('ps', 5)])
                        sch.op('act', lambda e, i2=i2, pv=pv: e.activation(out=vsb[i2][:], in_=pv[:, :], func=AF.Identity),
                               reads=[('ps', 4), ('ps', 5)], writes=[('vsb', i2)])
                        sch.dma('sp', vs[tt * 128:(tt + 1) * 128, :], vsb[i2][:], reads=[('vsb', i2)], writes=[('vs', tt)])
                        pr = self.ps[3]

                        def mmr(e, t=t, base=2048, pr=pr):
                            ins = None
                            for n in range(2):
                                for c in range(8):
                                    ins = e.matmul(pr[:, n * 512:(n + 1) * 512], hT[:, c, t * 128:(t + 1) * 128],
                                                   win[:, c, base + n * 512: base + (n + 1) * 512], start=(c == 0), stop=(c == 7))
                            return ins
                        sch.op('pe', mmr, reads=hk + ['win'], writes=[('ps', 6), ('ps', 7)])
                        sch.op('act', lambda e, i2=i2, pr=pr: e.activation(out=srsb[i2][:], in_=pr[:, :], func=AF.Silu),
                               reads=[('ps', 6), ('ps', 7)], writes=[('srsb', i2)])
                        sch.op('pool', lambda e, i2=i2: e.tensor_tensor(out=srgsb[i2][:], in0=srsb[i2][:], in1=gnB[:], op=ALU.mult),
                               reads=[('srsb', i2), 'gnB'], writes=[('srgsb', i2)])
                        sch.dma('sp', srgs[tt * 128:(tt + 1) * 128, :], srgsb[i2][:], reads=[('srgsb', i2)], writes=[('srgs', tt)])
                sch.barrier()
            with contextlib.ExitStack() as es:
                wout = self.sb(es, "wout", [128, 8, D], BF16)
                wo = 'm_wout%d' % li
                self.wload3(wout, wo, ['wout'])
                gB = self.load_grow(es, li, 0)
                R = self.residual_tiles(es, gB)
                state = self.sb(es, "state", [128, D], F32)
                stb = [self.sb(es, "stb", [128, D], BF16) for _ in range(2)]
                qT = [self.sb(es, "qT", [128, 4, 512], BF16) for _ in range(2)]
                kd = [self.sb(es, "kd", [64, 8, 512], BF16) for _ in range(2)]
                vv = [self.sb(es, "vv", [64, 8, D], BF16) for _ in range(2)]
                srg = [self.sb(es, "srg", [64, 8, D], BF16) for _ in range(2)]
                of = [self.sb(es, "of", [64, D], F32) for _ in range(2)]
                OT = self.sb(es, "OT", [128, 8, 512], BF16)
                ssq = self.sb(es, "gssq", [64, 4], F32)
                junk = self.sb(es, "gjunk", [64, 256], F32)
                sch.op('dve', lambda e: e.memset(state[:], 0.0), writes=['state'])
                k = 0
                for blk in range(NB):
                    i = blk % 2
                    sch.dma('sp', qT[i][:], qTs[:, :, blk * 512:(blk + 1) * 512].rearrange("h p s -> p h s"), writes=[('qT', i)])
                    sch.dma('sp', kd[i][:], kds[blk * 512:(blk + 1) * 512, :].rearrange("(n p) f -> p n f", p=64), writes=[('kd', i)])
                    sch.dma('sp', vv[i][:], vs[blk * 512:(blk + 1) * 512, :].rearrange("(n p) f -> p n f", p=64), writes=[('vv', i)])
                    sch.dma('sp', srg[i][:], srgs[blk * 512:(blk + 1) * 512, :].rearrange("(n p) f -> p n f", p=64), writes=[('srg', i)])
                    for n in range(8):
                        gn = blk * 8 + n
                        kk = k % 2
                        k += 1
                        pu = self.ps[0]

                        def mmu(e, i=i, n=n, pu=pu):
                            ins = None
                            for hd in range(4):
                                ins = e.matmul(pu[:, hd * 256:(hd + 1) * 256], kd[i][0:64, n, hd * 128:(hd + 1) * 128],
                                               vv[i][0:64, n, hd * 256:(hd + 1) * 256], start=True, stop=True)
                            return ins
                        sch.op('pe', mmu, reads=[('kd', i), ('vv', i)], writes=[('ps', 0), ('ps', 1)])

                        def upd(e, gn=gn, pu=pu):
                            ins = None
                            for hd in range(4):
                                ins = e.scalar_tensor_tensor(out=state[:, hd * 256:(hd + 1) * 256], in0=state[:, hd * 256:(hd + 1) * 256],
                                                             scalar=decay[:, hd, gn:gn + 1], in1=pu[:, hd * 256:(hd + 1) * 256],
                                                             op0=ALU.mult, op1=ALU.add)
                            return ins
                        sch.op('dve', upd, reads=[('ps', 0), ('ps', 1), 'decay', 'state'], writes=['state'])
                        sch.op('act', lambda e, kk=kk: e.activation(out=stb[kk][:], in_=state[:], func=AF.Identity),
                               reads=['state'], writes=[('stb', kk)])
                        po_ = self.ps[1]

                        def mmo(e, i=i, n=n, kk=kk, po_=po_):
                            ins = None
                            for hd in range(4):
                                ins = e.matmul(po_[0:64, hd * 256:(hd + 1) * 256], qT[i][:, hd, n * 64:(n + 1) * 64],
                                               stb[kk][:, hd * 256:(hd + 1) * 256], start=True, stop=True)
                            return ins
                        sch.op('pe', mmo, reads=[('qT', i), ('stb', kk)], writes=[('ps', 2), ('ps', 3)])

                        def sqs(e, po_=po_):
                            ins = None
                            for hd in range(4):
                                ins = e.activation(out=junk[0:64, :], in_=po_[0:64, hd * 256:(hd + 1) * 256], func=AF.Square,
                                                   accum_out=ssq[0:64, hd:hd + 1])
                            return ins
                        sch.op('act', sqs, reads=[('ps', 2), ('ps', 3)], writes=['gjunk', 'gssq'])
                        self.rstd_ops(ssq[0:64, 0:4], ssq[0:64, 0:4], 1.0 / 256.0, 'gssq', 'gssq')

                        def fin(e, i=i, n=n, kk=kk, po_=po_):
                            ins = None
                            for hd in range(4):
                                ins = e.scalar_tensor_tensor(out=of[kk][0:64, hd * 256:(hd + 1) * 256], in0=po_[0:64, hd * 256:(hd + 1) * 256],
                                                             scalar=ssq[0:64, hd:hd + 1], in1=srg[i][0:64, n, hd * 256:(hd + 1) * 256],
                                                             op0=ALU.mult, op1=ALU.mult)
                            return ins
                        sch.op('dve', fin, reads=[('ps', 2), ('ps', 3), 'gssq', ('srg', i)], writes=[('of', kk)])
                        p6, o6 = self.bank(6)

                        def trs(e, kk=kk, p6=p6, o6=o6):
                            ins = None
                            for c in range(8):
                                ins = e.transpose(out=p6[:, o6 + c * 64:o6 + (c + 1) * 64], in_=of[kk][0:64, c * 128:(c + 1) * 128],
                                                  identity=self.ident[0:64, 0:64])
                            return ins
                        sch.op('pe', trs, reads=[('of', kk), 'ident'], writes=[('ps', 6)])
                        sch.op('act', lambda e, n=n, p6=p6, o6=o6: e.activation(
                            out=OT[:, :, n * 64:(n + 1) * 64], in_=p6[:, o6:o6 + 512].rearrange("p (c t) -> p c t", c=8), func=AF.Identity),
                            reads=[('ps', 6)], writes=['OT'])
                    self.out_proj2(OT, ['OT'], wout, 4, blk * 512, R, src)
                sch.barrier()

    def conv_phase(self, li, src):
        sch = self.sch
        S = self.S
        NB = S // 512
        wn = 'm_win%d' % li
        import os
        if os.environ.get('CONV_STOP') == '0':
            return
        with contextlib.ExitStack() as esAB:
            uT = self.sb(esAB, "uT", [128, 8, S + 64], BF16)
            with contextlib.ExitStack() as es:
                T = self.prenorm_tiles(es)
                hT = self.sb(es, "hT", [128, 8, 512], BF16)
                win = self.sb(es, "win", [128, 8, 2048], BF16)
                bin_ = self.sb(es, "bin", [128, 16], F32)
                sg = [self.sb(es, "sg", [128, 512], F32) for _ in range(2)]
                self.wload3(win, wn, ['win'])
                if not os.environ.get('CONV_NOBIN'):
                    sch.dma('sp', bin_[:], self.W('m_bin%d' % li), reads=[self.wk(wn)], writes=['bin'])
                CS = os.environ.get('CONV_STOP', '')
                if CS != 'A1':
                    sch.op('dve', lambda e: e.memset(uT[:, :, 0:30], 0.0), writes=['uT'])
                for blk in range(NB):
                    self.prenorm_block(T, src, blk * 512, 4, li, 0, hT, 'hT')
                    hk = [('hT', c) for c in range(8)]
                    for m in range(8 if CS not in ('A1', 'A2') else 0):
                        ba, bg = (0, 1) if m % 2 == 0 else (2, 3)
                        pa, oa = self.bank(ba)
                        pg, og = self.bank(bg)

                        def mm(e, m=m, pa=pa, oa=oa, pg=pg, og=og):
                            ins = None
                            for c in range(8):
                                ins = e.matmul(pa[:, oa:oa + 512], win[:, c, m * 128:(m + 1) * 128], hT[:, c, :], start=(c == 0), stop=(c == 7))
                            for c in range(8):
                                ins = e.matmul(pg[:, og:og + 512], win[:, c, 1024 + m * 128:1024 + (m + 1) * 128], hT[:, c, :],
                                               start=(c == 0), stop=(c == 7))
                            return ins
                        sch.op('pe', mm, reads=hk + ['win'], writes=[('ps', ba), ('ps', bg)])
                        s_ = sg[m % 2]
                        if CS == 'A3':
                            continue
                        sch.op('act', lambda e, m=m, s_=s_, pg=pg, og=og: e.activation(out=s_[:], in_=pg[:, og:og + 512], func=AF.Sigmoid,
                                                                                     bias=bin_[:, 8 + m:9 + m], scale=1.0),
                               reads=[('ps', bg), 'bin'], writes=[('sg', m % 2)])
                        sch.op('dve', lambda e, m=m, s_=s_, pa=pa, oa=oa, blk=blk: e.scalar_tensor_tensor(
                            out=uT[:, m, 30 + blk * 512: 30 + (blk + 1) * 512], in0=pa[:, oa:oa + 512], scalar=bin_[:, m:m + 1], in1=s_[:],
                            op0=ALU.add, op1=ALU.mult),
                            reads=[('ps', ba), 'bin', ('sg', m % 2)], writes=[('uT', blk)])
                sch.barrier()
            import os
            if os.environ.get('CONV_STOP', '').startswith('A'):
                return
            with contextlib.ExitStack() as es:
                diag = self.sb(es, "diag", [128, 248, 128], BF16)
                dwT = self.sb(es, "dwT", [128, 248], F32)
                dwb = self.sb(es, "dwb", [1, D], BF16)
                bout = self.sb(es, "bout", [1, D], BF16)
                lng = self.sb(es, "lng", [128, 8], F32)
                lnb = self.sb(es, "lnb", [128, 8], F32)
                wout = self.sb(es, "wout", [128, 8, D], BF16)
                wo = 'm_wout%d' % li
                self.wload3(wout, wo, ['wout'])
                sch.dma('sp', dwT[:], self.W('m_dw%d' % li), reads=[self.wk(wo)], writes=['dwT'])
                sch.dma('pool', dwb[:], self.W('m_dwb%d' % li), reads=[self.wk(wo)], writes=['dwb'])
                sch.dma('pool', bout[:], self.W('m_bout%d' % li), reads=[self.wk(wo)], writes=['brow'])
                sch.dma('sp', lng[:], self.W('m_lng%d' % li), reads=[self.wk(wo)], writes=['lng'])
                sch.dma('sp', lnb[:], self.W('m_lnb%d' % li), reads=[self.wk(wo)], writes=['lnb'])
                gB = self.load_grow(es, li, 0)
                R = self.residual_tiles(es, gB)
                vn = self.sb(es, "vn", [128, 4, D], F32)
                sT = self.sb(es, "sT", [128, 8, 512], BF16)
                st = self.sb(es, "cst", [128, 8], F32)
                junk = self.sb(es, "cjunk", [128, D], F32)
                for kq in range(248):
                    sch.op('act', lambda e, kq=kq: e.activation(out=diag[:, kq, :], in_=self.identb[:], func=AF.Identity,
                                                                scale=dwT[:, kq:kq + 1]),
                           reads=['identb', 'dwT'], writes=['diag'])
                for blk in range(NB):
                    for t in range(4):
                        tt = blk * 4 + t
                        pv = self.ps[0]

                        def mmc(e, tt=tt, pv=pv):
                            ins = None
                            for c in range(8):
                                for j in range(31):
                                    ins = e.matmul(pv[:, c * 128:(c + 1) * 128], uT[:, c, tt * 128 + j: tt * 128 + j + 128],
                                                   diag[:, c * 31 + j, :], start=(j == 0), stop=False)
                                ins = e.matmul(pv[:, c * 128:(c + 1) * 128], self.onesb[0:1, 0:128], dwb[0:1, c * 128:(c + 1) * 128],
                                               start=False, stop=True)
                            return ins
                        sch.op('pe', mmc, reads=[('uT', b_) for b_ in range(NB)] + ['uT', 'diag', 'dwb', 'onesb'],
                               writes=[('ps', 0), ('ps', 1)])
                        sch.op('act', lambda e, pv=pv: e.activation(out=junk[:], in_=pv[:, :], func=AF.Identity, accum_out=st[:, 0:1]),
                               reads=[('ps', 0), ('ps', 1)], writes=['cjunk', 'cst'])
                        sch.op('act', lambda e, pv=pv: e.activation(out=junk[:], in_=pv[:, :], func=AF.Square, accum_out=st[:, 1:2]),
                               reads=[('ps', 0), ('ps', 1)], writes=['cjunk', 'cst'])

                        sch.op('dve', lambda e: e.tensor_scalar(out=st[:, 2:3], in0=st[:, 0:1], scalar1=1.0 / D, scalar2=None, op0=ALU.mult),
                               reads=['cst'], writes=['cst'])
                        sch.op('dve', lambda e: e.tensor_tensor(out=st[:, 3:4], in0=st[:, 2:3], in1=st[:, 2:3], op=ALU.mult),
                               reads=['cst'], writes=['cst'])
                        sch.op('dve', lambda e: e.scalar_tensor_tensor(out=st[:, 4:5], in0=st[:, 1:2], scalar=1.0 / D, in1=st[:, 3:4],
                                                                       op0=ALU.mult, op1=ALU.subtract), reads=['cst'], writes=['cst'])
                        self.rstd_ops(st[:, 5:6], st[:, 4:5], 1.0, 'cst', 'cst')
                        sch.op('dve', lambda e: e.scalar_tensor_tensor(out=st[:, 6:7], in0=st[:, 2:3], scalar=-1.0, in1=st[:, 5:6],
                                                                       op0=ALU.mult, op1=ALU.mult), reads=['cst'], writes=['cst'])
                        sch.op('act', lambda e, t=t, pv=pv: e.activation(out=vn[:, t, :], in_=pv[:, :], func=AF.Identity,
                                                                        scale=st[:, 5:6], bias=st[:, 6:7]),
                               reads=[('ps', 0), ('ps', 1), 'cst'], writes=[('vn', t)])
                    for c in range(8):
                        b = 6 + (c % 2)
                        pt, po = self.bank(b)

                        def tr(e, c=c, pt=pt, po=po):
                            ins = None
                            for t in range(4):
                                ins = e.transpose(out=pt[:, po + t * 128: po + (t + 1) * 128], in_=vn[:, t, c * 128:(c + 1) * 128],
                                                  identity=self.ident[:])
                            return ins
                        sch.op('pe', tr, reads=[('vn', t) for t in range(4)] + ['ident'], writes=[('ps', b)])
                        sch.op('act', lambda e, c=c, pt=pt, po=po: e.activation(out=sT[:, c, :], in_=pt[:, po:po + 512], func=AF.Silu,
                                                                               scale=lng[:, c:c + 1], bias=lnb[:, c:c + 1]),
                               reads=[('ps', b), 'lng', 'lnb'], writes=[('sT', c)])
                    self.out_proj2(sT, [('sT', c) for c in range(8)], wout, 4, blk * 512, R, src, bias_row=bout)
                sch.barrier()

    def fox_phase(self, li, src):
        sch = self.sch
        S = self.S
        NB = S // 512
        NTt = S // 128
        qTs = self.dscr("fqTs", [8, 128, S])
        kTs = self.dscr("fkTs", [8, 128, S])
        sogs = self.dscr("sogs", [S, 1024])
        wn = 'm_win%d' % li
        with contextlib.ExitStack() as esAB:
            Vaug = self.sb(esAB, "Vaug", [128, NTt, 16, 65], BF16)
            cum3 = self.sb(esAB, "cum3", [96, S], BF16)
            cposT = self.sb(esAB, "cposT", [128, NTt, 16], F32)
            with contextlib.ExitStack() as es:
                T = self.prenorm_tiles(es)
                hT = self.sb(es, "hT", [128, 8, 512], BF16)
                win = self.sb(es, "win", [128, 8, 4112], BF16)
                bones = self.sb(es, "bones", [128, 128], F32)
                nbf = self.sb(es, "nbf", [16, 1], F32)
                qg = self.sb(es, "qg", [128, 1], F32)
                kg = self.sb(es, "kg", [128, 1], F32)
                sq = [self.sb(es, "sq", [128, 512], F32) for _ in range(2)]
                rs = [self.sb(es, "rs", [128, 512], F32) for _ in range(2)]
                qk = [self.sb(es, "qk", [128, 512], BF16) for _ in range(2)]
                sog = [self.sb(es, "sog", [128, D], BF16) for _ in range(2)]
                cpos = self.sb(es, "cpos", [16, 512], F32)
                carry = self.sb(es, "carry", [16, 1], F32)
                ones16 = self.sb(es, "ones16", [16, 512], F32)
                fe = self.sb(es, "fe", [16, 512], F32)
                fl = fe
                r0 = self.sb(es, "r0", [16, 512], F32)
                hml = [self.sb(es, "hml", [16, 512], BF16) for _ in range(3)]
                self.wload3(win, wn, ['win'])
                sch.dma('sp', bones[:], self.consts[:, C_BONES:C_BONES + 128], writes=['bones'])
                sch.dma('sp', nbf[:], self.W('m_bf%d' % li), reads=[self.wk(wn)], writes=['nbf'])
                sch.dma('sp', qg[:], self.W('m_qg%d' % li), reads=[self.wk(wn)], writes=['qg'])
                sch.dma('sp', kg[:], self.W('m_kg%d' % li), reads=[self.wk(wn)], writes=['kg'])
                sch.op('dve', lambda e: e.tensor_scalar(out=nbf[:], in0=nbf[:], scalar1=-1.0, scalar2=None, op0=ALU.mult), reads=['nbf'], writes=['nbf'])
                sch.op('dve', lambda e: e.tensor_scalar(out=qg[:], in0=qg[:], scalar1=0.125, scalar2=None, op0=ALU.mult), reads=['qg'], writes=['qg'])
                sch.op('dve', lambda e: e.memset(ones16[:], 1.0), writes=['ones16'])
                sch.op('pool', lambda e: e.memset(Vaug[:], 1.0), writes=['Vaug'])
                sch.op('pool', lambda e: e.memset(cum3[:], 0.0), writes=['cum3'])
                mi = 0
                for blk in range(NB):
                    self.prenorm_block(T, src, blk * 512, 4, li, 0, hT, 'hT')
                    hk = [('hT', c) for c in range(8)]
                    for m in range(16):
                        i2 = mi % 2
                        mi += 1
                        b1, b2 = (0, 1) if i2 == 0 else (2, 3)
                        p1, o1 = self.bank(b1)
                        p2, o2 = self.bank(b2)

                        def mm(e, m=m, p1=p1, o1=o1):
                            ins = None
                            for c in range(8):
                                ins = e.matmul(p1[:, o1:o1 + 512], win[:, c, m * 128:(m + 1) * 128], hT[:, c, :], start=(c == 0), stop=(c == 7))
                            return ins
                        sch.op('pe', mm, reads=hk + ['win'], writes=[('ps', b1)])
                        sch.op('act', lambda e, i2=i2, p1=p1, o1=o1: e.activation(out=sq[i2][:], in_=p1[:, o1:o1 + 512], func=AF.Square),
                               reads=[('ps', b1)], writes=[('sq', i2)])
                        sch.op('pe', lambda e, i2=i2, p2=p2, o2=o2: e.matmul(p2[:, o2:o2 + 512], bones[:, :], sq[i2][:], start=True, stop=True),
                               reads=['bones', ('sq', i2)], writes=[('ps', b2)])
                        self.rstd_ops(rs[i2][:], p2[:, o2:o2 + 512], 1.0 / 64.0, ('ps', b2), ('rs', i2))
                        g_ = qg if m < 8 else kg
                        sch.op('dve', lambda e, i2=i2, p1=p1, o1=o1, g_=g_: e.scalar_tensor_tensor(
                            out=qk[i2][:], in0=p1[:, o1:o1 + 512], scalar=g_[:, 0:1], in1=rs[i2][:], op0=ALU.mult, op1=ALU.mult),
                            reads=[('ps', b1), ('rs', i2), 'qg', 'kg'], writes=[('qk', i2)])
                        dst = qTs if m < 8 else kTs
                        sch.dma('sp', dst[m % 8, :, blk * 512:(blk + 1) * 512], qk[i2][:], reads=[('qk', i2)], writes=[('qkTs', blk)])
                    for t in range(4):
                        tt = blk * 4 + t
                        pv = self.ps[2]

                        def mmv(e, t=t, base=2048, pv=pv):
                            ins = None
                            for n in range(2):
                                for c in range(8):
                                    ins = e.matmul(pv[:, n * 512:(n + 1) * 512], hT[:, c, t * 128:(t + 1) * 128],
                                                   win[:, c, base + n * 512: base + (n + 1) * 512], start=(c == 0), stop=(c == 7))
                            return ins
                        sch.op('pe', mmv, reads=hk + ['win'], writes=[('ps', 4), ('ps', 5)])
                        sch.op('act', lambda e, tt=tt, pv=pv: e.activation(out=Vaug[:, tt, :, 0:64], in_=pv[:, :].rearrange("p (h d) -> p h d", h=16),
                                                                          func=AF.Identity),
                               reads=[('ps', 4), ('ps', 5)], writes=['Vaug'])
                        pr = self.ps[3]

                        def mmo(e, t=t, base=3088, pr=pr):
                            ins = None
                            for n in range(2):
                                for c in range(8):
                                    ins = e.matmul(pr[:, n * 512:(n + 1) * 512], hT[:, c, t * 128:(t + 1) * 128],
                                                   win[:, c, base + n * 512: base + (n + 1) * 512], start=(c == 0), stop=(c == 7))
                            return ins
                        sch.op('pe', mmo, reads=hk + ['win'], writes=[('ps', 6), ('ps', 7)])
                        sch.op('act', lambda e, t=t, pr=pr: e.activation(out=sog[t % 2][:], in_=pr[:, :], func=AF.Sigmoid),
                               reads=[('ps', 6), ('ps', 7)], writes=[('sog', t % 2)])
                        sch.dma('sp', sogs[tt * 128:(tt + 1) * 128, :], sog[t % 2][:], reads=[('sog', t % 2)], writes=[('sogs', tt)])
                    p3, o3 = self.bank(3)

                    def mmf(e, p3=p3, o3=o3):
                        ins = None
                        for c in range(8):
                            ins = e.matmul(p3[0:16, o3:o3 + 512], win[:, c, 3072:3088], hT[:, c, :], start=(c == 0), stop=(c == 7))
                        return ins
                    sch.op('pe', mmf, reads=hk + ['win'], writes=[('ps', 3)])
                    sch.op('act', lambda e, p3=p3, o3=o3: e.activation(out=fe[:], in_=p3[0:16, o3:o3 + 512], func=AF.Exp, scale=-1.0, bias=nbf[0:16, :]),
                           reads=[('ps', 3), 'nbf'], writes=['fe'])
                    sch.op('act', lambda e: e.activation(out=fl[:], in_=fe[:], func=AF.Ln, bias=self.onec[0:16, :], scale=1.0),
                           reads=['fe', 'onec'], writes=['fe'])
                    init = 0.0 if blk == 0 else carry[:, 0:1]
                    sch.op('dve', lambda e, blk=blk, init=init: e.tensor_tensor_scan(
                        out=cpos[:, :], data0=ones16[:], data1=fl[:], initial=init, op0=ALU.mult, op1=ALU.add),
                        reads=['fe', 'ones16', 'carry'], writes=['cpos'])
                    sch.op('dve', lambda e: e.tensor_copy(out=carry[:, 0:1], in_=cpos[:, 511:512]), reads=['cpos'], writes=['carry'])

                    sch.op('dve', lambda e, blk=blk: e.tensor_scalar(out=r0[:], in0=cpos[:, :], scalar1=-1.0, scalar2=None,
                                                                    op0=ALU.mult), reads=['cpos'], writes=['r0'])
                    for j in range(3):
                        sch.op('dve', lambda e, j=j: e.tensor_copy(out=hml[j][:], in_=r0[:]), reads=['r0'], writes=[('hml', j)])
                        if j < 2:
                            sch.op('dve', lambda e, j=j: e.tensor_tensor(out=r0[:], in0=r0[:], in1=hml[j][:], op=ALU.subtract),
                                   reads=['r0', ('hml', j)], writes=['r0'])
                    for j in range(3):
                        sch.dma('sp', cum3[32 * j:32 * j + 16, blk * 512:(blk + 1) * 512], hml[j][:], reads=[('hml', j)], writes=['cum3'])
                    for t in range(4):
                        tt = blk * 4 + t
                        p0, o0 = self.bank(t % 2)
                        sch.op('pe', lambda e, t=t, p0=p0, o0=o0: e.transpose(out=p0[:, o0:o0 + 16], in_=cpos[0:16, t * 128:(t + 1) * 128],
                                                                              identity=self.ident[0:16, 0:16]),
                               reads=['cpos', 'ident'], writes=[('ps', t % 2)])
                        sch.op('dve', lambda e, tt=tt, p0=p0, o0=o0: e.tensor_copy(out=cposT[:, tt, :], in_=p0[:, o0:o0 + 16]),
                               reads=[('ps', t % 2)], writes=['cposT'])
                sch.barrier()
            import os
            if os.environ.get('FOX_STOP') == 'A':
                return
            with contextlib.ExitStack() as es:
                sel = self.sb(es, "sel", [96, 16, 128], BF16)
                maskb = self.sb(es, "maskb", [128, 128], BF16)
                wout = self.sb(es, "wout", [128, 8, D], BF16)
                wo = 'm_wout%d' % li
                self.wload3(wout, wo, ['wout'])
                sch.dma('pool', sel[:], self.consts[0:96, C_SEL:C_SEL + 2048].rearrange("p (h m) -> p h m", h=16), writes=['sel'])
                sch.dma('pool', maskb[:], self.consts[:, C_MASK:C_MASK + 128], writes=['maskb'])
                gB = self.load_grow(es, li, 0)
                R = self.residual_tiles(es, gB)
                qT = [self.sb(es, "fqT", [128, 8, 512], BF16) for _ in range(2)]
                kT = [self.sb(es, "fkT", [128, S], BF16) for _ in range(2)]
                sg = self.sb(es, "fsog", [128, 4, D], BF16)
                O = self.sb(es, "fO", [128, 4, D], F32)
                PT = [self.sb(es, "PT", [128, 512], BF16) for _ in range(3)]
                rc = self.sb(es, "rc", [128, 4], F32)
                OT = self.sb(es, "fOT", [128, 8, 512], BF16)
                ki = 0
                pi = 0
                for qb in range(NB):
                    i = qb % 2
                    nk = (qb + 1) * 512
                    sch.dma('sp', qT[i][:], qTs[:, :, qb * 512:(qb + 1) * 512].rearrange("m p s -> p m s"), writes=[('fqT', i)])
                    sch.dma('sp', sg[:], sogs[qb * 512:(qb + 1) * 512, :].rearrange("(t p) f -> p t f", p=128), writes=['fsog'])
                    for m in range(8):
                        j = ki % 2
                        ki += 1
                        sch.dma('sp', kT[j][:, 0:nk], kTs[m, :, 0:nk], writes=[('fkT', j)])
                        for hh in range(2):
                            h = 2 * m + hh
                            pb = hh * 64
                            ab = 3 if h % 2 == 0 else 7
                            pa, oa = self.bank(ab)
                            nkt = 4 * qb + 4
                            for kt in range(nkt):
                                q0 = max(0, kt - 4 * qb)
                                qc0 = q0 * 128
                                sb_ = kt % 3
                                pst, ost = self.bank(sb_)
                                k3 = pi % 3
                                pi += 1
                                diagt = kt >= 4 * qb

                                def mms(e, i=i, j=j, m=m, h=h, pb=pb, kt=kt, qc0=qc0, pst=pst, ost=ost, diagt=diagt, qb=qb):
                                    lk = kT[j][pb:pb + 64, kt * 128:(kt + 1) * 128]
                                    if diagt:
                                        e.matmul(pst[:, ost + qc0:ost + qc0 + 128], lk, qT[i][pb:pb + 64, m, qc0:qc0 + 128], start=True, stop=False)
                                        e.matmul(pst[:, ost + qc0:ost + qc0 + 128], sel[0:96, h, :], cum3[0:96, qb * 512 + qc0: qb * 512 + qc0 + 128],
                                                 start=False, stop=False)
                                        ins = e.matmul(pst[:, ost + qc0:ost + qc0 + 128], self.identb[:, :], maskb[:, :], start=False, stop=True)
                                        lo = qc0 + 128
                                    else:
                                        lo = qc0
                                        ins = None
                                    if lo < 512:
                                        e.matmul(pst[:, ost + lo:ost + 512], lk, qT[i][pb:pb + 64, m, lo:512], start=True, stop=False)
                                        ins = e.matmul(pst[:, ost + lo:ost + 512], sel[0:96, h, :], cum3[0:96, qb * 512 + lo:(qb + 1) * 512],
                                                       start=False, stop=True)
                                    return ins
                                sch.op('pe', mms, reads=[('fkT', j), ('fqT', i), 'sel', 'cum3', 'identb', 'maskb'], writes=[('ps', sb_)])
                                sch.op('act', lambda e, k3=k3, kt=kt, h=h, qc0=qc0, pst=pst, ost=ost: e.activation(
                                    out=PT[k3][:, qc0:512], in_=pst[:, ost + qc0:ost + 512], func=AF.Exp, bias=cposT[:, kt, h:h + 1], scale=1.0),
                                    reads=[('ps', sb_), 'cposT'], writes=[('PT', k3)])

                                def mmpv(e, k3=k3, kt=kt, h=h, q0=q0, qb=qb, pa=pa, oa=oa):
                                    ins = None
                                    for qs in range(q0, 4):
                                        ins = e.matmul(pa[:, oa + qs * 65: oa + qs * 65 + 65], PT[k3][:, qs * 128:(qs + 1) * 128],
                                                       Vaug[:, kt, h, :], start=(kt == 0), stop=(kt == 4 * qb + qs))
                                    return ins
                                sch.op('pe', mmpv, reads=[('PT', k3), 'Vaug'], writes=[('ps', ab)])

                            sch.op('dve', lambda e, pa=pa, oa=oa: e.reciprocal(
                                out=rc[:, 0:4], in_=pa[:, oa:oa + 260].rearrange("p (q d) -> p q d", q=4)[:, :, 64]),
                                reads=[('ps', ab)], writes=['rc'])

                            def norm(e, h=h, pa=pa, oa=oa):
                                ins = None
                                for qs in range(4):
                                    ins = e.tensor_scalar(out=O[:, qs, h * 64:(h + 1) * 64], in0=pa[:, oa + qs * 65: oa + qs * 65 + 64],
                                                          scalar1=rc[:, qs:qs + 1], scalar2=None, op0=ALU.mult)
                                return ins
                            sch.op('dve', norm, reads=[('ps', ab), 'rc'], writes=[('fO', h)] + [('fOg', t_) for t_ in range(4)])
                    for t in range(4):
                        sch.op('pool', lambda e, t=t: e.tensor_tensor(out=O[:, t, :], in0=O[:, t, :], in1=sg[:, t, :], op=ALU.mult),
                               reads=[('fO', h_) for h_ in range(16)] + ['fsog'], writes=[('fOg', t)])
                    for c in range(8):
                        b = 6 if c % 2 == 0 else 2
                        pt, po = self.bank(b)

                        def tr(e, c=c, pt=pt, po=po):
                            ins = None
                            for t in range(4):
                                ins = e.transpose(out=pt[:, po + t * 128: po + (t + 1) * 128], in_=O[:, t, c * 128:(c + 1) * 128],
                                                  identity=self.ident[:])
                            return ins
                        sch.op('pe', tr, reads=[('fOg', t) for t in range(4)] + ['ident'], writes=[('ps', b)])
                        sch.op('act', lambda e, c=c, pt=pt, po=po: e.activation(out=OT[:, c, :], in_=pt[:, po:po + 512], func=AF.Identity),
                               reads=[('ps', b)], writes=[('fOT', c)])
                    self.out_proj2(OT, [('fOT', c) for c in range(8)], wout, 4, qb * 512, R, src)
                sch.barrier()

_CACHE = {}


def run_cfg(cfg, inputs):
    key = repr(sorted(cfg.items(), key=lambda kv: kv[0]))
    if key not in _CACHE:
        _CACHE[key] = Prog(cfg).build()
    nc = _CACHE[key]
    n = cfg.get('NCORE', NCORES)
    nseq = cfg.get('NSEQ', 1)
    coll = cfg.get('COLL', True)
    S = cfg['S']
    packed = pack_weights(cfg, inputs)
    consts = make_consts()
    x = np.asarray(inputs['x'], dtype=np.float32)
    c = np.asarray(inputs['c'], dtype=np.float32)
    assert n * nseq == x.shape[0]
    if not coll:
        full = [np.ascontiguousarray(pk.reshape(-1, WC)) for pk in packed]
    in_maps = []
    for b in range(n):
        m = {"x": np.ascontiguousarray(x[b * nseq:(b + 1) * nseq].reshape(nseq * S, D)),
             "cvec": np.ascontiguousarray(np.concatenate([colform(c[b * nseq + q]) for q in range(nseq)], axis=1)),
             "consts": consts}
        for g, pk in enumerate(packed):
            m["wsh%d" % g] = pk[b] if coll else full[g]
        in_maps.append(m)
    res = run_bass_kernel_spmd(nc, in_maps, core_ids=list(range(n)))
    return np.concatenate([res.results[b]["y"].reshape(nseq, S, D) for b in range(n)], axis=0)


FULL_CFG = dict(S=4096, layers=[('gla', 'ffn'), ('conv', 'moe'), ('fox', 'ffn'), ('gla', 'moe')],
                DFF=2816, DFFE=3584, E=8, NCORE=2, NSEQ=4, COLL=False)


def kernel(**inputs):
    return run_cfg(FULL_CFG, inputs).astype(np.float32)
```

```python
import contextlib
import numpy as np
import concourse.bass as bass
import concourse.mybir as mybir
from concourse.bass_utils import run_bass_kernel_spmd

F32 = mybir.dt.float32
BF16 = mybir.dt.bfloat16
AF = mybir.ActivationFunctionType
ALU = mybir.AluOpType

D = 1024
EPS = 1e-6
WC = 2048
NCORES = 8

C_ID, C_TRIU, C_CIND, C_BONES, C_MASK, C_SEL = 0, 128, 256, 258, 386, 514
NCONST = 514 + 16 * 128


def make_consts():
    c = np.zeros((128, NCONST), np.float32)
    p = np.arange(128)
    c[:, C_ID:C_ID + 128] = np.eye(128, dtype=np.float32)
    c[:, C_TRIU:C_TRIU + 128] = ((p[:, None] > p[None, :]) & (p[:, None] // 64 == p[None, :] // 64))
    c[:, C_CIND + 0] = (p // 64 == 0)
    c[:, C_CIND + 1] = (p // 64 == 1)
    c[:, C_BONES:C_BONES + 128] = (p[:, None] // 64 == p[None, :] // 64)
    c[:, C_MASK:C_MASK + 128] = np.where(p[:, None] <= p[None, :], 0.0, -30000.0)
    for h in range(16):
        for r in (h, 32 + h, 64 + h):
            c[r, C_SEL + h * 128:C_SEL + (h + 1) * 128] = 1.0
    return c


class Sched:
    CE = ('pe', 'act', 'dve', 'pool')
    DQ = ('sp', 'pool', 'act')

    def __init__(self, nc, es, nd=8):
        self.nc = nc
        self.nd = nd
        self.eng = dict(pe=nc.tensor, act=nc.scalar, dve=nc.vector, pool=nc.gpsimd, sp=nc.sync)
        self.csem = {e: es.enter_context(nc.semaphore('c_' + e)) for e in self.CE}
        self.cnt = {e: 0 for e in self.CE}
        self.dsem = {q: [es.enter_context(nc.semaphore('d_%s%d' % (q, i))) for i in range(nd)] for q in self.DQ}
        self.dcnt = {q: 0 for q in self.DQ}
        self.ccsem = es.enter_context(nc.semaphore('ccs'))
        self.cccnt = 0
        self.waited = {e: {} for e in self.eng}
        self.drained = {e: 0 for e in self.CE}
        self.lw = {}
        self.rd = {}
        self.nins = 0

    def _wait(self, E, tok):
        sem, val, F, kind = tok
        w = self.waited[E]
        if w.get(sem.name, 0) >= val:
            return
        self.eng[E].wait_ge(sem, val)
        w[sem.name] = val

    def _deps(self, E, reads, writes):
        toks = []
        for r in reads:
            t = self.lw.get(r)
            if t is not None:
                toks.append((t, True))
        for wk in writes:
            t = self.lw.get(wk)
            if t is not None:
                toks.append((t, True))
            for t in self.rd.get(wk, {}).values():
                toks.append((t, False))
        for t, strong in toks:
            sem, val, F, kind = t
            if kind == 'c' and F == E:
                if E == 'pe' or not strong:
                    continue
                if self.drained[E] < val:
                    self.eng[E].drain()
                    self.drained[E] = self.cnt[E]
                continue
            self._wait(E, t)

    def _commit(self, tok, reads, writes):
        for r in reads:
            d = self.rd.setdefault(r, {})
            d[tok[0].name] = tok
        for wk in writes:
            self.lw[wk] = tok
            self.rd[wk] = {}

    def op(self, E, fn, reads=(), writes=()):
        self._deps(E, reads, writes)
        ins = fn(self.eng[E])
        self.cnt[E] += 1
        ins.then_inc(self.csem[E], 1)
        self._commit((self.csem[E], self.cnt[E], E, 'c'), reads, writes)
        self.nins += 1

    def dma(self, Q, out, in_, reads=(), writes=()):
        j = self.dcnt[Q]
        slot = j % self.nd
        sem = self.dsem[Q][slot]
        if j >= self.nd:
            self._wait(Q, (sem, 16 * (j // self.nd), Q, 'dma'))
        self._deps(Q, reads, writes)
        self.eng[Q].dma_start(out=out, in_=in_).then_inc(sem, 16)
        self.dcnt[Q] = j + 1
        self._commit((sem, 16 * (j // self.nd + 1), Q, 'dma'), reads, writes)

    def collective(self, in_ap, out_ap, reads=(), writes=()):
        self._deps('pool', reads, writes)
        self.nc.gpsimd.collective_compute(
            "AllGather", ALU.bypass, replica_groups=[list(range(NCORES))],
            ins=[in_ap], outs=[out_ap]).then_inc(self.ccsem)
        self.cccnt += 1
        self._commit((self.ccsem, self.cccnt, 'pool', 'cc'), reads, writes)

    def barrier(self):
        toks = [(self.csem[e], self.cnt[e], e, 'c') for e in self.CE if self.cnt[e] > 0]
        for q in self.DQ:
            j = self.dcnt[q]
            for slot in range(self.nd):
                n = (j - slot + self.nd - 1) // self.nd if j > slot else 0
                if n > 0:
                    toks.append((self.dsem[q][slot], 16 * n, q, 'dma'))
        for E in self.eng:
            for t in toks:
                if t[3] == 'c' and t[2] == E:
                    if self.drained[E] < t[1]:
                        self.eng[E].drain()
                        self.drained[E] = self.cnt[E]
                    continue
                self._wait(E, t)
        keep = {k: t for k, t in self.lw.items() if t[3] == 'cc'}
        self.lw.clear()
        self.rd.clear()
        self.lw.update(keep)


def pmaj(w):
    K, F = w.shape
    return np.ascontiguousarray(w.reshape(K // 128, 128, F).transpose(1, 0, 2)).reshape(128, -1)


def colform(v):
    return np.ascontiguousarray(np.asarray(v).reshape(-1, 128).T)


def ffn_units(F):
    nj = F // 128
    out = []
    j = 0
    while j < nj:
        n = min(4, nj - j)
        out.append((j, n))
        j += n
    return out


def pack_ffn(w13, w2, F):
    parts = []
    for (j0, n) in ffn_units(F):
        parts.append(pmaj(w13[:, j0 * 128:(j0 + n) * 128]))
        parts.append(pmaj(w13[:, F + j0 * 128:F + (j0 + n) * 128]))
        parts.append(pmaj(w2[j0 * 128:(j0 + n) * 128, :]))
    return np.concatenate(parts, axis=1)


def weight_plan(cfg):
    plan = []
    L = cfg['layers']
    DFF, DFFE, E = cfg['DFF'], cfg['DFFE'], cfg['E']
    cnt = {}
    for i, (mx, ff) in enumerate(L):
        g = i
        plan.append((g, 'ada_w%d' % i, 128, 8 * 6144, lambda I, i=i: pmaj(I['ada_w'][i])))
        plan.append((g, 'ada_bc%d' % i, 128, 48, lambda I, i=i: colform(I['ada_b'][i])))
        plan.append((g, 'ada_br%d' % i, 1, 6144, lambda I, i=i: np.asarray(I['ada_b'][i]).reshape(1, -1)))
        j = cnt.get(mx, 0)
        cnt[mx] = j + 1
        if mx == 'gla':
            plan.append((g, 'm_win%d' % i, 128, 8 * 3088, lambda I, j=j: pmaj(I['gla_w_in'][j])))
            plan.append((g, 'm_wg2%d' % i, 17, 512, lambda I, j=j: np.concatenate(
                [I['gla_w_gate2'][j], np.asarray(I['gla_b_gate'][j]).reshape(1, -1)], axis=0)))
            plan.append((g, 'm_gng%d' % i, 1, 1024, lambda I, j=j: np.asarray(I['gla_gn_g'][j]).reshape(1, -1)))
            plan.append((g, 'm_wout%d' % i, 128, 8 * 1024, lambda I, j=j: pmaj(I['gla_w_out'][j])))
        elif mx == 'conv':
            plan.append((g, 'm_win%d' % i, 128, 8 * 2048, lambda I, j=j: pmaj(I['conv_w_in'][j])))
            plan.append((g, 'm_bin%d' % i, 128, 16, lambda I, j=j: colform(I['conv_b_in'][j])))
            plan.append((g, 'm_dw%d' % i, 128, 8 * 31, lambda I, j=j: np.ascontiguousarray(
                np.asarray(I['conv_dw'][j]).T.reshape(8, 128, 31).transpose(1, 0, 2)).reshape(128, -1)))
            plan.append((g, 'm_dwb%d' % i, 1, 1024, lambda I, j=j: np.asarray(I['conv_dw_b'][j]).reshape(1, -1)))
            plan.append((g, 'm_lng%d' % i, 128, 8, lambda I, j=j: colform(I['conv_ln_g'][j])))
            plan.append((g, 'm_lnb%d' % i, 128, 8, lambda I, j=j: colform(I['conv_ln_b'][j])))
            plan.append((g, 'm_wout%d' % i, 128, 8 * 1024, lambda I, j=j: pmaj(I['conv_w_out'][j])))
            plan.append((g, 'm_bout%d' % i, 1, 1024, lambda I, j=j: np.asarray(I['conv_b_out'][j]).reshape(1, -1)))
        elif mx == 'fox':
            plan.append((g, 'm_win%d' % i, 128, 8 * 4112, lambda I, j=j: pmaj(I['fox_w_in'][j])))
            plan.append((g, 'm_bf%d' % i, 16, 1, lambda I, j=j: np.asarray(I['fox_b_f'][j]).reshape(16, 1)))
            plan.append((g, 'm_qg%d' % i, 128, 1, lambda I, j=j: np.tile(np.asarray(I['fox_qn_g'][j]), 2).reshape(128, 1)))
            plan.append((g, 'm_kg%d' % i, 128, 1, lambda I, j=j: np.tile(np.asarray(I['fox_kn_g'][j]), 2).reshape(128, 1)))
            plan.append((g, 'm_wout%d' % i, 128, 8 * 1024, lambda I, j=j: pmaj(I['fox_w_out'][j])))
        k = cnt.get(ff, 0)
        cnt[ff] = k + 1
        if ff == 'ffn':
            plan.append((g, 'f_w%d' % i, 128, 24 * DFF, lambda I, k=k: pack_ffn(I['ffn_w13'][k], I['ffn_w2'][k], DFF)))
        elif ff == 'moe':
            plan.append((g, 'f_rt%d' % i, 128, 8 * E, lambda I, k=k: pmaj(I['moe_router'][k])))
            for e in range(E):
                plan.append((g, 'f_w%d_%d' % (i, e), 128, 24 * DFFE,
                             lambda I, k=k, e=e: pack_ffn(I['moe_w13'][k][e], I['moe_w2'][k][e], DFFE)))
    g = len(L) - 1 if L else 0
    plan.append((g, 'nf_g', 1, 1024, lambda I: np.asarray(I['norm_f_g']).reshape(1, -1)))
    return plan


def plan_layout(cfg):
    plan = weight_plan(cfg)
    ngroups = max(len(cfg['layers']), 1)
    offs = [0] * ngroups
    layout = {}
    for (g, name, P, X, fn) in plan:
        layout[name] = (g, offs[g], P, X)
        offs[g] += (P * X + 63) // 64 * 64
    rows = []
    for g in range(ngroups):
        per = NCORES * WC
        rows.append(max((offs[g] + per - 1) // per, 1))
    return plan, layout, rows


def pack_weights(cfg, inputs):
    plan, layout, rows = plan_layout(cfg)
    bufs = [np.zeros((NCORES * r * WC,), np.float32) for r in rows]
    for (g, name, P, X, fn) in plan:
        a = np.asarray(fn(inputs), dtype=np.float32)
        assert a.shape == (P, X), (name, a.shape, P, X)
        off = layout[name][1]
        bufs[g][off:off + P * X] = a.reshape(-1)
    return [b.reshape(NCORES, r, WC) for b, r in zip(bufs, rows)]


class Prog:
    def __init__(self, cfg):
        self.cfg = cfg
        self.S = cfg['S']
        self.uid = 0

    def nm(self, base):
        self.uid += 1
        return '%s_%d' % (base, self.uid)

    def sb(self, es, base, shape, dt):
        return es.enter_context(self.nc.sbuf_tensor(self.nm(base), list(shape), dt, align_bytes=128))

    def bank(self, b):
        t = self.ps[b // 2]
        o = (b % 2) * 512
        return t, o

    def W(self, name):
        g, off, P, X = self.layout[name]
        flat = self.wflat[g]
        return flat[off:off + P * X].rearrange("(p x) -> p x", p=P)

    def wk(self, name):
        return ('wall', self.layout[name][0])

    def wload3(self, tile, name, writes):
        src = self.W(name).rearrange("p (c f) -> p c f", c=8)
        for c in range(8):
            self.sch.dma('pool', tile[:, c, :], src[:, c, :], reads=[self.wk(name)], writes=list(writes))

    def build(self):
        cfg = self.cfg
        S = self.S
        nc = bass.Bass("TRN2", target_bir_lowering=False)
        self.nc = nc
        self.plan, self.layout, self.rows = plan_layout(cfg)
        ng = len(self.rows)
        NSEQ = cfg.get('NSEQ', 1)
        COLL = cfg.get('COLL', True)
        self.x_all = nc.dram_tensor("x", [NSEQ * S, D], F32, kind="ExternalInput").ap()
        self.cvec_all = nc.dram_tensor("cvec", [128, 8 * NSEQ], F32, kind="ExternalInput").ap()
        self.consts = nc.dram_tensor("consts", [128, NCONST], F32, kind="ExternalInput").ap()
        self.y_all = nc.dram_tensor("y", [NSEQ * S, D], F32, kind="ExternalOutput").ap()
        if COLL:
            wsh = [nc.dram_tensor("wsh%d" % g, [self.rows[g], WC], F32, kind="ExternalInput") for g in range(ng)]
            bounce = [nc.dram_tensor("wb%d" % g, [self.rows[g], WC], F32) for g in range(ng)]
            wall = [nc.dram_tensor("wall%d" % g, [NCORES * self.rows[g], WC], F32) for g in range(ng)]
        else:
            wall = [nc.dram_tensor("wsh%d" % g, [NCORES * self.rows[g], WC], F32, kind="ExternalInput") for g in range(ng)]
        self.wflat = [w.ap().rearrange("a b -> (a b)") for w in wall]
        self.xs = nc.dram_tensor("xs", [S, D], F32).ap()
        self.modrow = nc.dram_tensor("modrow", [2 * max(len(cfg['layers']), 1), D], F32).ap()
        self.scr = {}

        with contextlib.ExitStack() as es:
            self.ges = es
            sch = Sched(nc, es)
            self.sch = sch
            self.ps = [es.enter_context(nc.psum_tensor("ps%d" % i, [128, 1024], F32)) for i in range(4)]
            self.ident = self.sb(es, "ident", [128, 128], F32)
            self.identb = self.sb(es, "identb", [128, 128], BF16)
            self.onesb = self.sb(es, "onesb", [128, 128], BF16)
            self.onec = self.sb(es, "onec", [128, 1], F32)
            self.epsc = self.sb(es, "epsc", [128, 1], F32)
            self.modc = self.sb(es, "modc", [128, max(len(cfg['layers']), 1) * 32], F32)
            sch.dma('sp', self.ident[:], self.consts[:, C_ID:C_ID + 128], writes=['ident'])
            sch.op('dve', lambda e: e.tensor_copy(out=self.identb[:], in_=self.ident[:]), reads=['ident'], writes=['identb'])
            sch.op('dve', lambda e: e.memset(self.onesb[:], 1.0), writes=['onesb'])
            sch.op('dve', lambda e: e.memset(self.onec[:], 1.0), writes=['onec'])
            sch.op('dve', lambda e: e.memset(self.epsc[:], EPS), writes=['epsc'])
            if COLL:
                for g in range(ng):
                    sch.dma('pool', bounce[g][:, :], wsh[g][:, :], writes=[('wb', g)])
                    sch.collective(bounce[g][:, :], wall[g][:, :], reads=[('wb', g)], writes=[('wall', g)])
            sch.barrier()
            for sq in range(NSEQ):
                self.x_in = self.x_all[sq * S:(sq + 1) * S, :]
                self.y = self.y_all[sq * S:(sq + 1) * S, :]
                self.cvec = self.cvec_all[:, sq * 8:(sq + 1) * 8]
                self.adaln_phase()
                first = True
                for i, (mx, ff) in enumerate(cfg['layers']):
                    src = self.x_in if first else self.xs
                    if mx == 'gla':
                        self.gla_phase(i, src)
                        first = False
                    elif mx == 'conv':
                        self.conv_phase(i, src)
                        first = False
                    elif mx == 'fox':
                        self.fox_phase(i, src)
                        first = False
                    src = self.x_in if first else self.xs
                    if ff in ('ffn', 'moe'):
                        self.ffn_phase(i, src, ff == 'moe')
                        first = False
                self.final_phase(self.x_in if first else self.xs)
            sch.barrier()
        return nc

    def rstd_ops(self, out_ap, in_ap, inv_n, key_in, key_out):
        sch = self.sch
        P = out_ap.shape[0]
        sch.op('act', lambda e: e.activation(out=out_ap, in_=in_ap, func=AF.Sqrt, scale=inv_n, bias=self.epsc[0:P, :]),
               reads=[key_in, 'epsc'], writes=[key_out])
        sch.op('dve', lambda e: e.reciprocal(out=out_ap, in_=out_ap), reads=[key_out], writes=[key_out])

    def prenorm_block(self, T, src, tok0, ntile, li, which, hT, hT_key, hT32=None):
        sch = self.sch
        sc = self.modc[:, li * 32 + which * 16 + 8: li * 32 + which * 16 + 16]
        sh = self.modc[:, li * 32 + which * 16: li * 32 + which * 16 + 8]
        xn = T['xn']
        for t in range(ntile):
            xt = T['xt'][t % len(T['xt'])]
            kx = ('xt', t % len(T['xt']))
            sch.dma('sp', xt[:], src[tok0 + t * 128: tok0 + (t + 1) * 128, :],
                    reads=[('xs', (tok0 // 128) + t)], writes=[kx])
            ssq = T['ssq']
            sch.op('act', lambda e, xt=xt, t=t: e.activation(out=T['junk'][:], in_=xt[:], func=AF.Square,
                                                             accum_out=ssq[:, t:t + 1]),
                   reads=[kx], writes=['junk', ('ssq', t)])
            self.rstd_ops(ssq[:, t:t + 1], ssq[:, t:t + 1], 1.0 / D, ('ssq', t), ('ssq', t))
            sch.op('dve', lambda e, xt=xt, t=t: e.tensor_scalar(out=xn[:, t, :], in0=xt[:], scalar1=ssq[:, t:t + 1],
                                                                scalar2=None, op0=ALU.mult),
                   reads=[kx, ('ssq', t)], writes=[('xn', t)])
        nb = T['tbanks']
        for c in range(8):
            b = nb[c % len(nb)]
            pt, po = self.bank(b)

            def tr(e, c=c, pt=pt, po=po):
                ins = None
                for t in range(ntile):
                    ins = e.transpose(out=pt[:, po + t * 128: po + (t + 1) * 128],
                                      in_=xn[:, t, c * 128:(c + 1) * 128], identity=self.ident[:])
                return ins
            sch.op('pe', tr, reads=[('xn', t) for t in range(ntile)] + ['ident'], writes=[('ps', b)])
            if hT32 is not None:
                sch.op('act', lambda e, c=c, pt=pt, po=po: e.activation(
                    out=hT32[:, c, 0:ntile * 128], in_=pt[:, po:po + ntile * 128], func=AF.Identity,
                    scale=sc[:, c:c + 1], bias=sh[:, c:c + 1]),
                    reads=[('ps', b), 'modc'], writes=[('hT32', c)])
                sch.op('pool', lambda e, c=c: e.tensor_copy(out=hT[:, c, 0:ntile * 128], in_=hT32[:, c, 0:ntile * 128]),
                       reads=[('hT32', c)], writes=[(hT_key, c)])
            else:
                sch.op('act', lambda e, c=c, pt=pt, po=po: e.activation(
                    out=hT[:, c, 0:ntile * 128], in_=pt[:, po:po + ntile * 128], func=AF.Identity,
                    scale=sc[:, c:c + 1], bias=sh[:, c:c + 1]),
                    reads=[('ps', b), 'modc'], writes=[(hT_key, c)])

    def prenorm_tiles(self, es, nxt=2):
        T = {}
        T['xt'] = [self.sb(es, "xt", [128, D], F32) for _ in range(nxt)]
        T['xn'] = self.sb(es, "xn", [128, 4, D], F32)
        T['junk'] = self.sb(es, "junk", [128, D], F32)
        T['ssq'] = self.sb(es, "ssq", [128, 4], F32)
        T['tbanks'] = [6, 7]
        return T

    def load_grow(self, es, li, which):
        gB = self.sb(es, "gB", [128, D], F32)
        r = li * 2 + which
        self.sch.dma('sp', gB[:], self.modrow[r:r + 1, :].to_broadcast([128, D]), reads=['modrow'], writes=['gB'])
        return gB

    def residual_store(self, R, ykey, ypt, tok0, src):
        sch = self.sch
        i = R['i'] % len(R['xr'])
        R['i'] += 1
        xr = R['xr'][i]
        tmp = R['tmp'][i]
        kt = ('xs', tok0 // 128)
        sch.dma('sp', xr[:], src[tok0:tok0 + 128, :], reads=[kt], writes=[('xr', i)])
        sch.op('dve', lambda e: e.tensor_tensor(out=tmp[:], in0=ypt, in1=R['gB'][:], op=ALU.mult),
               reads=list(ykey) + ['gB'], writes=[('rtmp', i)])
        sch.op('pool', lambda e: e.tensor_tensor(out=tmp[:], in0=tmp[:], in1=xr[:], op=ALU.add),
               reads=[('rtmp', i), ('xr', i)], writes=[('rtmp', i)])
        sch.dma('sp', self.xs[tok0:tok0 + 128, :], tmp[:], reads=[('rtmp', i)], writes=[kt])

    def residual_tiles(self, es, gB):
        return dict(i=0, gB=gB, xr=[self.sb(es, "xr", [128, D], F32) for _ in range(2)],
                    tmp=[self.sb(es, "rtmp", [128, D], F32) for _ in range(2)])

    def out_proj(self, OT, otkey, wout, ntile, tok0, R, src, bias_row=None):
        sch = self.sch
        pt = self.ps[2]
        for t in range(ntile):
            def mm(e, t=t):
                ins = None
                for n in range(2):
                    for c in range(8):
                        ins = e.matmul(pt[:, n * 512:(n + 1) * 512], OT[:, c, t * 128:(t + 1) * 128],
                                       wout[:, c, n * 512:(n + 1) * 512], start=(c == 0),
                                       stop=(c == 7 and bias_row is None))
                    if bias_row is not None:
                        ins = e.matmul(pt[:, n * 512:(n + 1) * 512], self.onesb[0:1, 0:128],
                                       bias_row[0:1, n * 512:(n + 1) * 512], start=False, stop=True)
                return ins
            sch.op('pe', mm, reads=[(otkey, c) for c in range(8)] + ['wout', 'brow', 'onesb'],
                   writes=[('ps', 4), ('ps', 5)])
            self.residual_store(R, [('ps', 4), ('ps', 5)], pt[:, :], tok0 + t * 128, src)

    def adaln_phase(self):
        sch = self.sch
        nc = self.nc
        L = self.cfg['layers']
        with contextlib.ExitStack() as es:
            cc = self.sb(es, "cc", [128, 8], F32)
            cb = self.sb(es, "cb", [128, 8], BF16)
            wA = [self.sb(es, "wA", [128, 8, 1024], BF16) for _ in range(2)]
            bc = self.sb(es, "bc", [128, 48], F32)
            br = self.sb(es, "br", [1, 6144], F32)
            row = self.sb(es, "row", [1, 1024], F32)
            sch.dma('sp', cc[:], self.cvec[:, :], writes=['cc'])
            sch.op('act', lambda e: e.activation(out=cc[:], in_=cc[:], func=AF.Silu), reads=['cc'], writes=['cc'])
            sch.op('dve', lambda e: e.tensor_copy(out=cb[:], in_=cc[:]), reads=['cc'], writes=['cb'])
            k = 0
            for i in range(len(L)):
                sch.dma('sp', bc[:], self.W('ada_bc%d' % i), reads=[self.wk('ada_bc%d' % i)], writes=['bc'])
                sch.dma('sp', br[:], self.W('ada_br%d' % i), reads=[self.wk('ada_br%d' % i)], writes=['br'])
                aw = self.W('ada_w%d' % i).rearrange("p (c f) -> p c f", c=8)
                for g in range(6):
                    w = wA[k % 2]
                    kw = ('wA', k % 2)
                    k += 1
                    for c0 in (0, 4):
                        sch.dma('pool', w[:, c0:c0 + 4, :], aw[:, c0:c0 + 4, g * 1024:(g + 1) * 1024], reads=[self.wk('ada_w%d' % i)], writes=[kw])
                    pt, po = self.bank(0)
                    if g in (0, 1, 3, 4):
                        def mm(e, w=w, pt=pt, po=po):
                            ins = None
                            for oc in range(8):
                                for c in range(8):
                                    ins = e.matmul(pt[:, po + oc:po + oc + 1], w[:, c, oc * 128:(oc + 1) * 128],
                                                   cb[:, c:c + 1], start=(c == 0), stop=(c == 7))
                            return ins
                        sch.op('pe', mm, reads=[kw, 'cb'], writes=[('ps', 0)])
                        which = 0 if g < 2 else 1
                        isscale = g in (1, 4)
                        dst = self.modc[:, i * 32 + which * 16 + (8 if isscale else 0): i * 32 + which * 16 + (16 if isscale else 8)]
                        sch.op('dve', lambda e, dst=dst, pt=pt, po=po, g=g: e.tensor_tensor(
                            out=dst, in0=pt[:, po:po + 8], in1=bc[:, g * 8:(g + 1) * 8], op=ALU.add),
                            reads=[('ps', 0), 'bc'], writes=['modc'])
                        if isscale:
                            sch.op('dve', lambda e, dst=dst: e.tensor_scalar(out=dst, in0=dst, scalar1=1.0, scalar2=None,
                                                                             op0=ALU.add), reads=['modc'], writes=['modc'])
                    else:
                        which = 0 if g == 2 else 1
                        for n in range(2):
                            def mm(e, w=w, pt=pt, po=po, n=n):
                                ins = None
                                for c in range(8):
                                    ins = e.matmul(pt[0:1, po:po + 512], cb[:, c:c + 1], w[:, c, n * 512:(n + 1) * 512],
                                                   start=(c == 0), stop=(c == 7))
                                return ins
                            sch.op('pe', mm, reads=[kw, 'cb'], writes=[('ps', 0)])
                            sch.op('dve', lambda e, pt=pt, po=po, n=n, g=g: e.tensor_tensor(
                                out=row[0:1, n * 512:(n + 1) * 512], in0=pt[0:1, po:po + 512],
                                in1=br[0:1, g * 1024 + n * 512: g * 1024 + (n + 1) * 512], op=ALU.add),
                                reads=[('ps', 0), 'br'], writes=['row'])
                        r = i * 2 + which
                        sch.dma('sp', self.modrow[r:r + 1, :], row[0:1, :], reads=['row'], writes=['modrow'])
            sch.barrier()

    def ffn_phase(self, li, src, moe):
        sch = self.sch
        cfg = self.cfg
        S = self.S
        F = cfg['DFFE'] if moe else cfg['DFF']
        E = cfg['E'] if moe else 1
        units = ffn_units(F)
        TB = min(1024, S)
        NT = TB // 128
        NQ = TB // 512
        with contextlib.ExitStack() as es:
            T = self.prenorm_tiles(es)
            hT = self.sb(es, "hT", [128, 8, TB], BF16)
            hT32 = self.sb(es, "hT32", [128, 8, 512], F32) if moe else None
            acc = self.sb(es, "acc", [128, NT, D], F32)
            act = [self.sb(es, "act", [128, 4, TB], BF16) for _ in range(2)]
            sa = [self.sb(es, "sa", [128, 512], F32) for _ in range(2)]
            w13b = [self.sb(es, "w13b", [128, 8192], BF16) for _ in range(2)]
            w2b = [self.sb(es, "w2b", [128, 4096], BF16) for _ in range(2)]
            gB = self.load_grow(es, li, 1)
            R = self.residual_tiles(es, gB)
            if moe:
                rt = self.sb(es, "rt", [128, 8, E], F32)
                G = self.sb(es, "G", [128, NT, E], F32)
                lg = self.sb(es, "lg", [128, E], F32)
                ex = self.sb(es, "ex", [128, E], F32)
                mx8 = self.sb(es, "mx8", [128, 8], F32)
                lg8 = self.sb(es, "lg8", [128, 8], F32)
                sm = self.sb(es, "sm", [128, 4], F32)
                sch.dma('sp', rt[:], self.W('f_rt%d' % li).rearrange("p (c e) -> p c e", c=8), reads=[self.wk('f_rt%d' % li)], writes=['rt'])
                if E < 8:
                    sch.op('dve', lambda e: e.memset(lg8[:], -1e30), writes=['lg8'])
            ui = 0
            si = 0
            for blk in range(S // TB):
                tok0 = blk * TB
                for q in range(NQ):
                    if moe:
                        hsub = self.sb(es, "hsub", [128, 8, 512], BF16) if False else None
                    hview = hT[:, :, q * 512:(q + 1) * 512]
                    self.prenorm_block(T, src, tok0 + q * 512, 4, li, 1, hview, ('hT', q), hT32=hT32)
                    if moe:
                        for t in range(4):
                            tt = q * 4 + t
                            pt, po = self.bank(6 + (t % 2))

                            def mm(e, pt=pt, po=po, t=t):
                                ins = None
                                for c in range(8):
                                    ins = e.matmul(pt[:, po:po + E], hT32[:, c, t * 128:(t + 1) * 128], rt[:, c, :],
                                                   start=(c == 0), stop=(c == 7))
                                return ins
                            kb = ('ps', 6 + (t % 2))
                            sch.op('pe', mm, reads=[('hT32', c) for c in range(8)] + ['rt'], writes=[kb])
                            sch.op('dve', lambda e, pt=pt, po=po: e.tensor_copy(out=lg8[:, 0:E], in_=pt[:, po:po + E]),
                                   reads=[kb], writes=['lg8'])
                            sch.op('dve', lambda e: e.max(out=mx8[:], in_=lg8[:]), reads=['lg8'], writes=['mx8'])
                            sch.op('dve', lambda e: e.tensor_scalar(out=sm[:, 0:1], in0=mx8[:, 0:1], scalar1=-1.0, scalar2=None,
                                                                    op0=ALU.mult), reads=['mx8'], writes=['sm'])
                            sch.op('act', lambda e: e.activation(out=ex[:], in_=lg8[:, 0:E], func=AF.Exp, bias=sm[:, 0:1], scale=1.0),
                                   reads=['lg8', 'sm'], writes=['ex'])
                            sch.op('dve', lambda e: e.scalar_tensor_tensor(out=ex[:], in0=lg8[:, 0:E], scalar=mx8[:, 1:2], in1=ex[:],
                                                                           op0=ALU.is_ge, op1=ALU.mult),
                                   reads=['lg8', 'mx8', 'ex'], writes=['ex'])
                            sch.op('dve', lambda e: e.reduce_sum(out=sm[:, 1:2], in_=ex[:], axis=mybir.AxisListType.X),
                                   reads=['ex'], writes=['sm'])
                            sch.op('dve', lambda e: e.reciprocal(out=sm[:, 2:3], in_=sm[:, 1:2]), reads=['sm'], writes=['sm'])
                            sch.op('dve', lambda e, tt=tt: e.tensor_scalar(out=G[:, tt, :], in0=ex[:], scalar1=sm[:, 2:3],
                                                                          scalar2=None, op0=ALU.mult),
                                   reads=['ex', 'sm'], writes=[('G', tt)])
                ulist = [(ex_i, j0, n) for ex_i in range(E) for (j0, n) in units]

                def emit_h1(idx, u):
                    nonlocal si
                    ex_i, j0, n = u
                    wname = ('f_w%d_%d' % (li, ex_i)) if moe else ('f_w%d' % li)
                    wv = self.W(wname)
                    w = w13b[idx % 2]
                    kw = ('w13', idx % 2)
                    w2t = w2b[idx % 2]
                    kw2 = ('w2b', idx % 2)
                    a_t = act[idx % 2]
                    ka = ('act', idx % 2)
                    base = j0 * 3072
                    for pc in range(2):
                        sch.dma('pool', w[:, pc * n * 1024:(pc + 1) * n * 1024], wv[:, base + pc * n * 1024: base + (pc + 1) * n * 1024],
                                reads=[self.wk(wname)], writes=[kw])
                    sch.dma('pool', w2t[:, 0:n * 1024], wv[:, base + 2 * n * 1024: base + 3 * n * 1024],
                            reads=[self.wk(wname)], writes=[kw2])
                    for jj in range(n):
                        for q in range(NQ):
                            ba, bb = (0, 1) if (si % 2 == 0) else (2, 3)
                            s_t = sa[si % 2]
                            ks = ('sa', si % 2)
                            si += 1
                            pa, oa = self.bank(ba)
                            pb, ob = self.bank(bb)

                            def mm(e, w=w, jj=jj, q=q, n=n, pa=pa, oa=oa, pb=pb, ob=ob):
                                ins = None
                                for c in range(8):
                                    ins = e.matmul(pa[:, oa:oa + 512], w[:, c * n * 128 + jj * 128: c * n * 128 + (jj + 1) * 128],
                                                   hT[:, c, q * 512:(q + 1) * 512], start=(c == 0), stop=(c == 7))
                                for c in range(8):
                                    o = 8 * n * 128
                                    ins = e.matmul(pb[:, ob:ob + 512], w[:, o + c * n * 128 + jj * 128: o + c * n * 128 + (jj + 1) * 128],
                                                   hT[:, c, q * 512:(q + 1) * 512], start=(c == 0), stop=(c == 7))
                                return ins
                            sch.op('pe', mm, reads=[kw] + [('hT', q)] + [(('hT', q), c) for c in range(8)],
                                   writes=[('ps', ba), ('ps', bb)])
                            sch.op('act', lambda e, s_t=s_t, pa=pa, oa=oa: e.activation(out=s_t[:], in_=pa[:, oa:oa + 512], func=AF.Silu),
                                   reads=[('ps', ba)], writes=[ks])
                            sch.op('dve', lambda e, s_t=s_t, pb=pb, ob=ob, a_t=a_t, jj=jj, q=q: e.tensor_tensor(
                                out=a_t[:, jj, q * 512:(q + 1) * 512], in0=s_t[:], in1=pb[:, ob:ob + 512], op=ALU.mult),
                                reads=[ks, ('ps', bb)], writes=[ka])

                def emit_y(idx, u):
                    ex_i, j0, n = u
                    w2t = w2b[idx % 2]
                    kw2 = ('w2b', idx % 2)
                    a_t = act[idx % 2]
                    ka = ('act', idx % 2)
                    firstu = (idx == 0)
                    for t in range(NT):
                        pt = self.ps[2]

                        def mm2(e, w2t=w2t, n=n, t=t, a_t=a_t, pt=pt):
                            ins = None
                            for nn in range(2):
                                for jj in range(n):
                                    ins = e.matmul(pt[:, nn * 512:(nn + 1) * 512], a_t[:, jj, t * 128:(t + 1) * 128],
                                                   w2t[:, jj * 1024 + nn * 512: jj * 1024 + (nn + 1) * 512],
                                                   start=(jj == 0), stop=(jj == n - 1))
                            return ins
                        sch.op('pe', mm2, reads=[kw2, ka], writes=[('ps', 4), ('ps', 5)])
                        if moe:
                            if firstu:
                                sch.op('dve', lambda e, t=t, pt=pt, ex_i=ex_i: e.tensor_scalar(
                                    out=acc[:, t, :], in0=pt[:, :], scalar1=G[:, t, ex_i:ex_i + 1], scalar2=None, op0=ALU.mult),
                                    reads=[('ps', 4), ('ps', 5), ('G', t)], writes=[('acc', t)])
                            else:
                                sch.op('dve', lambda e, t=t, pt=pt, ex_i=ex_i: e.scalar_tensor_tensor(
                                    out=acc[:, t, :], in0=pt[:, :], scalar=G[:, t, ex_i:ex_i + 1], in1=acc[:, t, :],
                                    op0=ALU.mult, op1=ALU.add),
                                    reads=[('ps', 4), ('ps', 5), ('G', t), ('acc', t)], writes=[('acc', t)])
                        else:
                            if firstu:
                                sch.op('act', lambda e, t=t, pt=pt: e.activation(out=acc[:, t, :], in_=pt[:, :], func=AF.Identity),
                                       reads=[('ps', 4), ('ps', 5)], writes=[('acc', t)])
                            else:
                                sch.op('dve', lambda e, t=t, pt=pt: e.tensor_tensor(out=acc[:, t, :], in0=pt[:, :], in1=acc[:, t, :],
                                                                                   op=ALU.add),
                                       reads=[('ps', 4), ('ps', 5), ('acc', t)], writes=[('acc', t)])

                for idx, u in enumerate(ulist):
                    emit_h1(idx, u)
                    if idx >= 1:
                        emit_y(idx - 1, ulist[idx - 1])
                emit_y(len(ulist) - 1, ulist[-1])
                for t in range(NT):
                    self.residual_store(R, [('acc', t)], acc[:, t, :], tok0 + t * 128, src)
            sch.barrier()

    def final_phase(self, src):
        sch = self.sch
        S = self.S
        with contextlib.ExitStack() as es:
            gB = self.sb(es, "nfg", [128, D], F32)
            sch.dma('sp', gB[:], self.W('nf_g').to_broadcast([128, D]), reads=[self.wk('nf_g')], writes=['nfg'])
            xt = [self.sb(es, "fxt", [128, D], F32) for _ in range(3)]
            junk = self.sb(es, "fjunk", [128, D], F32)
            ssq = self.sb(es, "fssq", [128, 4], F32)
            for tt in range(S // 128):
                i = tt % 3
                x = xt[i]
                kx = ('fxt', i)
                ks = ('fssq', tt % 4)
                sc = ssq[:, tt % 4: tt % 4 + 1]
                sch.dma('sp', x[:], src[tt * 128:(tt + 1) * 128, :], reads=[('xs', tt)], writes=[kx])
                sch.op('act', lambda e, x=x, sc=sc: e.activation(out=junk[:], in_=x[:], func=AF.Square, accum_out=sc),
                       reads=[kx], writes=['fjunk', ks])
                self.rstd_ops(sc, sc, 1.0 / D, ks, ks)
                sch.op('dve', lambda e, x=x, sc=sc: e.scalar_tensor_tensor(out=x[:], in0=x[:], scalar=sc, in1=gB[:],
                                                                          op0=ALU.mult, op1=ALU.mult),
                       reads=[kx, ks, 'nfg'], writes=[kx])
                sch.dma('sp', self.y[tt * 128:(tt + 1) * 128, :], x[:], reads=[kx], writes=[('y', tt)])
            sch.barrier()

    def dscr(self, base, shape, dt=BF16):
        if base not in self.scr:
            self.scr[base] = self.nc.dram_tensor(self.nm(base), list(shape), dt).ap()
        return self.scr[base]

    def out_proj2(self, OT, otkeys, wout, ntile, tok0, R, src, bias_row=None):
        sch = self.sch
        pt = self.ps[2]
        for t in range(ntile):
            def mm(e, t=t):
                ins = None
                for n in range(2):
                    for c in range(8):
                        ins = e.matmul(pt[:, n * 512:(n + 1) * 512], OT[:, c, t * 128:(t + 1) * 128],
                                       wout[:, c, n * 512:(n + 1) * 512], start=(c == 0),
                                       stop=(c == 7 and bias_row is None))
                    if bias_row is not None:
                        ins = e.matmul(pt[:, n * 512:(n + 1) * 512], self.onesb[0:1, 0:128],
                                       bias_row[0:1, n * 512:(n + 1) * 512], start=False, stop=True)
                return ins
            sch.op('pe', mm, reads=list(otkeys) + ['wout', 'brow', 'onesb'], writes=[('ps', 4), ('ps', 5)])
            self.residual_store(R, [('ps', 4), ('ps', 5)], pt[:, :], tok0 + t * 128, src)

    def gla_phase(self, li, src):
        sch = self.sch
        S = self.S
        NB = S // 512
        NCHK = S // 64
        qTs = self.dscr("qTs%d" % li, [4, 128, S])
        kds = self.dscr("kds%d" % li, [S, 512])
        vs = self.dscr("vs%d" % li, [S, 1024])
        srgs = self.dscr("srgs%d" % li, [S, 1024])
        wn = 'm_win%d' % li
        with contextlib.ExitStack() as esAB:
            decay = self.sb(esAB, "decay", [128, 4, NCHK], F32)
            with contextlib.ExitStack() as es:
                T = self.prenorm_tiles(es)
                hT = self.sb(es, "hT", [128, 8, 512], BF16)
                win = self.sb(es, "win", [128, 8, 3088], BF16)
                wg2 = self.sb(es, "wg2", [17, 512], F32)
                gnB = self.sb(es, "gnB", [128, D], F32)
                triu = self.sb(es, "triu", [128, 128], F32)
                cind = self.sb(es, "cind", [128, 2], F32)
                glr = self.sb(es, "glr", [17, 512], F32)
                e1 = [self.sb(es, "e1", [128, 512], F32) for _ in range(2)]
                nla = [self.sb(es, "nla", [128, 512], F32) for _ in range(2)]
                dec = [self.sb(es, "dec", [128, 512], F32) for _ in range(2)]
                qsb = [self.sb(es, "qsb", [128, 512], BF16) for _ in range(2)]
                kdsb = [self.sb(es, "kdsb", [128, 512], BF16) for _ in range(2)]
                vsb = [self.sb(es, "vsb", [128, D], BF16) for _ in range(2)]
                srsb = [self.sb(es, "srsb", [128, D], F32) for _ in range(2)]
                srgsb = [self.sb(es, "srgsb", [128, D], BF16) for _ in range(2)]
                self.wload3(win, wn, ['win'])
                sch.dma('sp', wg2[:], self.W('m_wg2%d' % li), reads=[self.wk(wn)], writes=['wg2'])
                sch.dma('sp', gnB[:], self.W('m_gng%d' % li).to_broadcast([128, D]), reads=[self.wk(wn)], writes=['gnB'])
                sch.dma('sp', triu[:], self.consts[:, C_TRIU:C_TRIU + 128], writes=['triu'])
                sch.dma('sp', cind[:], self.consts[:, C_CIND:C_CIND + 2], writes=['cind'])
                sch.op('dve', lambda e: e.memset(glr[:], 1.0), writes=['glr'])
                ri = 0
                for blk in range(NB):
                    self.prenorm_block(T, src, blk * 512, 4, li, 0, hT, 'hT')
                    hk = [('hT', c) for c in range(8)]
                    for hd in range(4):
                        b = hd % 2
                        pt, po = self.bank(b)

                        def mm(e, hd=hd, pt=pt, po=po):
                            ins = None
                            for c in range(8):
                                ins = e.matmul(pt[:, po:po + 512], win[:, c, hd * 128:(hd + 1) * 128], hT[:, c, :],
                                               start=(c == 0), stop=(c == 7))
                            return ins
                        sch.op('pe', mm, reads=hk + ['win'], writes=[('ps', b)])
                        q = qsb[hd % 2]
                        sch.op('act', lambda e, q=q, pt=pt, po=po: e.activation(out=q[:], in_=pt[:, po:po + 512], func=AF.Identity,
                                                                               scale=float(128 ** -0.5)),
                               reads=[('ps', b)], writes=[('qsb', hd % 2)])
                        sch.dma('sp', qTs[hd, :, blk * 512:(blk + 1) * 512], q[:], reads=[('qsb', hd % 2)], writes=[('qTs', blk)])
                    pt, po = self.bank(2)

                    def mmg(e, pt=pt, po=po):
                        ins = None
                        for c in range(8):
                            ins = e.matmul(pt[0:16, po:po + 512], win[:, c, 3072:3088], hT[:, c, :], start=(c == 0), stop=(c == 7))
                        return ins
                    sch.op('pe', mmg, reads=hk + ['win'], writes=[('ps', 2)])
                    sch.op('act', lambda e, pt=pt, po=po: e.activation(out=glr[0:16, :], in_=pt[0:16, po:po + 512], func=AF.Identity),
                           reads=[('ps', 2)], writes=['glr'])
                    for t in range(4):
                        tt = blk * 4 + t
                        i2 = ri % 2
                        ri += 1
                        p3, o3 = self.bank(3)
                        sch.op('pe', lambda e, t=t, p3=p3, o3=o3: e.matmul(p3[:, o3:o3 + 512], glr[0:17, t * 128:(t + 1) * 128], wg2[0:17, :],
                                                                        start=True, stop=True),
                               reads=['glr', 'wg2'], writes=[('ps', 3)])
                        sch.op('act', lambda e, i2=i2, p3=p3, o3=o3: e.activation(out=e1[i2][:], in_=p3[:, o3:o3 + 512], func=AF.Exp, scale=-1.0),
                               reads=[('ps', 3)], writes=[('e1', i2)])
                        sch.op('act', lambda e, i2=i2: e.activation(out=nla[i2][:], in_=e1[i2][:], func=AF.Ln, bias=self.onec[:, 0:1], scale=1.0),
                               reads=[('e1', i2), 'onec'], writes=[('nla', i2)])
                        p0, o0 = self.bank(0)
                        sch.op('pe', lambda e, i2=i2, p0=p0, o0=o0: e.matmul(p0[:, o0:o0 + 512], triu[:, :], nla[i2][:], start=True, stop=True),
                               reads=['triu', ('nla', i2)], writes=[('ps', 0)])
                        sch.op('act', lambda e, i2=i2, p0=p0, o0=o0: e.activation(out=dec[i2][:], in_=p0[:, o0:o0 + 512], func=AF.Exp, scale=-1.0 / 16.0),
                               reads=[('ps', 0)], writes=[('dec', i2)])
                        p1, o1 = self.bank(1)

                        def mmk(e, t=t, p1=p1, o1=o1):
                            ins = None
                            for c in range(8):
                                ins = e.matmul(p1[:, o1:o1 + 512], hT[:, c, t * 128:(t + 1) * 128], win[:, c, 512:1024],
                                               start=(c == 0), stop=(c == 7))
                            return ins
                        sch.op('pe', mmk, reads=hk + ['win'], writes=[('ps', 1)])
                        sch.op('dve', lambda e, i2=i2, p1=p1, o1=o1: e.tensor_tensor(out=kdsb[i2][:], in0=p1[:, o1:o1 + 512], in1=dec[i2][:], op=ALU.mult),
                               reads=[('ps', 1), ('dec', i2)], writes=[('kdsb', i2)])
                        sch.dma('sp', kds[tt * 128:(tt + 1) * 128, :], kdsb[i2][:], reads=[('kdsb', i2)], writes=[('kds', tt)])
                        p2, o2 = self.bank(2)

                        def mmt(e, i2=i2, p2=p2, o2=o2):
                            ins = None
                            for hd in range(4):
                                ins = e.matmul(p2[:, o2 + hd * 2:o2 + hd * 2 + 2], nla[i2][:, hd * 128:(hd + 1) * 128], cind[:, 0:2],
                                               start=True, stop=True)
                            return ins
                        sch.op('pe', mmt, reads=[('nla', i2), 'cind'], writes=[('ps', 2)])

                        def dcy(e, tt=tt, p2=p2, o2=o2):
                            ins = None
                            for hd in range(4):
                                ins = e.activation(out=decay[:, hd, tt * 2:tt * 2 + 2], in_=p2[:, o2 + hd * 2:o2 + hd * 2 + 2],
                                                   func=AF.Exp, scale=-1.0 / 16.0)
                            return ins
                        sch.op('act', dcy, reads=[('ps', 2)], writes=['decay'])
                        pv = self.ps[2]

                        def mmv(e, t=t, base=1024, pv=pv):
                            ins = None
                            for n in range(2):
                                for c in range(8):
                                    ins = e.matmul(pv[:, n * 512:(n + 1) * 512], hT[:, c, t * 128:(t + 1) * 128],
                                                   win[:, c, base + n * 512: base + (n + 1) * 512], start=(c == 0), stop=(c == 7))
                            return ins
                        sch.op('pe', mmv, reads=hk + ['win'], writes=[('ps', 4), ('ps', 5)])
                        sch.op('act', lambda e, i2=i2, pv=pv: e.activation(out=vsb[i2][:], in_=pv[:, :], func=AF.Identity),
                               reads=[('ps', 4), ('ps', 5)], writes=[('vsb', i2)])
                        sch.dma('sp', vs[tt * 128:(tt + 1) * 128, :], vsb[i2][:], reads=[('vsb', i2)], writes=[('vs', tt)])
                        pr = self.ps[3]

                        def mmr(e, t=t, base=2048, pr=pr):
                            ins = None
                            for n in range(2):
                                for c in range(8):
                                    ins = e.matmul(pr[:, n * 512:(n + 1) * 512], hT[:, c, t * 128:(t + 1) * 128],
                                                   win[:, c, base + n * 512: base + (n + 1) * 512], start=(c == 0), stop=(c == 7))
                            return ins
                        sch.op('pe', mmr, reads=hk + ['win'], writes=[('ps', 6), ('ps', 7)])
                        sch.op('act', lambda e, i2=i2, pr=pr: e.activation(out=srsb[i2][:], in_=pr[:, :], func=AF.Silu),
                               reads=[('ps', 6), ('ps', 7)], writes=[('srsb', i2)])
                        sch.op('pool', lambda e, i2=i2: e.tensor_tensor(out=srgsb[i2][:], in0=srsb[i2][:], in1=gnB[:], op=ALU.mult),
                               reads=[('srsb', i2), 'gnB'], writes=[('srgsb', i2)])
                        sch.dma('sp', srgs[tt * 128:(tt + 1) * 128, :], srgsb[i2][:], reads=[('srgsb', i2)], writes=[('srgs', tt)])
                sch.barrier()
            with contextlib.ExitStack() as es:
                wout = self.sb(es, "wout", [128, 8, D], BF16)
                wo = 'm_wout%d' % li
                self.wload3(wout, wo, ['wout'])
                gB = self.load_grow(es, li, 0)
                R = self.residual_tiles(es, gB)
                state = self.sb(es, "state", [128, D], F32)
                stb = [self.sb(es, "stb", [128, D], BF16) for _ in range(2)]
                qT = [self.sb(es, "qT", [128, 4, 512], BF16) for _ in range(2)]
                kd = [self.sb(es, "kd", [64, 8, 512], BF16) for _ in range(2)]
                vv = [self.sb(es, "vv", [64, 8, D], BF16) for _ in range(2)]
                srg = [self.sb(es, "srg", [64, 8, D], BF16) for _ in range(2)]
                of = [self.sb(es, "of", [64, D], F32) for _ in range(2)]
                OT = self.sb(es, "OT", [128, 8, 512], BF16)
                ssq = self.sb(es, "gssq", [64, 4], F32)
                junk = self.sb(es, "gjunk", [64, 256], F32)
                sch.op('dve', lambda e: e.memset(state[:], 0.0), writes=['state'])
                k = 0
                for blk in range(NB):
                    i = blk % 2
                    sch.dma('sp', qT[i][:], qTs[:, :, blk * 512:(blk + 1) * 512].rearrange("h p s -> p h s"), writes=[('qT', i)])
                    sch.dma('sp', kd[i][:], kds[blk * 512:(blk + 1) * 512, :].rearrange("(n p) f -> p n f", p=64), writes=[('kd', i)])
                    sch.dma('sp', vv[i][:], vs[blk * 512:(blk + 1) * 512, :].rearrange("(n p) f -> p n f", p=64), writes=[('vv', i)])
                    sch.dma('sp', srg[i][:], srgs[blk * 512:(blk + 1) * 512, :].rearrange("(n p) f -> p n f", p=64), writes=[('srg', i)])
                    for n in range(8):
                        gn = blk * 8 + n
                        kk = k % 2
                        k += 1
                        pu = self.ps[0]

                        def mmu(e, i=i, n=n, pu=pu):
                            ins = None
                            for hd in range(4):
                                ins = e.matmul(pu[:, hd * 256:(hd + 1) * 256], kd[i][0:64, n, hd * 128:(hd + 1) * 128],
                                               vv[i][0:64, n, hd * 256:(hd + 1) * 256], start=True, stop=True)
                            return ins
                        sch.op('pe', mmu, reads=[('kd', i), ('vv', i)], writes=[('ps', 0), ('ps', 1)])

                        def upd(e, gn=gn, pu=pu):
                            ins = None
                            for hd in range(4):
                                ins = e.scalar_tensor_tensor(out=state[:, hd * 256:(hd + 1) * 256], in0=state[:, hd * 256:(hd + 1) * 256],
                                                             scalar=decay[:, hd, gn:gn + 1], in1=pu[:, hd * 256:(hd + 1) * 256],
                                                             op0=ALU.mult, op1=ALU.add)
                            return ins
                        sch.op('dve', upd, reads=[('ps', 0), ('ps', 1), 'decay', 'state'], writes=['state'])
                        sch.op('act', lambda e, kk=kk: e.activation(out=stb[kk][:], in_=state[:], func=AF.Identity),
                               reads=['state'], writes=[('stb', kk)])
                        po_ = self.ps[1]

                        def mmo(e, i=i, n=n, kk=kk, po_=po_):
                            ins = None
                            for hd in range(4):
                                ins = e.matmul(po_[0:64, hd * 256:(hd + 1) * 256], qT[i][:, hd, n * 64:(n + 1) * 64],
                                               stb[kk][:, hd * 256:(hd + 1) * 256], start=True, stop=True)
                            return ins
                        sch.op('pe', mmo, reads=[('qT', i), ('stb', kk)], writes=[('ps', 2), ('ps', 3)])

                        def sqs(e, po_=po_):
                            ins = None
                            for hd in range(4):
                                ins = e.activation(out=junk[0:64, :], in_=po_[0:64, hd * 256:(hd + 1) * 256], func=AF.Square,
                                                   accum_out=ssq[0:64, hd:hd + 1])
                            return ins
                        sch.op('act', sqs, reads=[('ps', 2), ('ps', 3)], writes=['gjunk', 'gssq'])
                        self.rstd_ops(ssq[0:64, 0:4], ssq[0:64, 0:4], 1.0 / 256.0, 'gssq', 'gssq')

                        def fin(e, i=i, n=n, kk=kk, po_=po_):
                            ins = None
                            for hd in range(4):
                                ins = e.scalar_tensor_tensor(out=of[kk][0:64, hd * 256:(hd + 1) * 256], in0=po_[0:64, hd * 256:(hd + 1) * 256],
                                                             scalar=ssq[0:64, hd:hd + 1], in1=srg[i][0:64, n, hd * 256:(hd + 1) * 256],
                                                             op0=ALU.mult, op1=ALU.mult)
                            return ins
                        sch.op('dve', fin, reads=[('ps', 2), ('ps', 3), 'gssq', ('srg', i)], writes=[('of', kk)])
                        p6, o6 = self.bank(6)

                        def trs(e, kk=kk, p6=p6, o6=o6):
                            ins = None
                            for c in range(8):
                                ins = e.transpose(out=p6[:, o6 + c * 64:o6 + (c + 1) * 64], in_=of[kk][0:64, c * 128:(c + 1) * 128],
                                                  identity=self.ident[0:64, 0:64])
                            return ins
                        sch.op('pe', trs, reads=[('of', kk), 'ident'], writes=[('ps', 6)])
                        sch.op('act', lambda e, n=n, p6=p6, o6=o6: e.activation(
                            out=OT[:, :, n * 64:(n + 1) * 64], in_=p6[:, o6:o6 + 512].rearrange("p (c t) -> p c t", c=8), func=AF.Identity),
                            reads=[('ps', 6)], writes=['OT'])
                    self.out_proj2(OT, ['OT'], wout, 4, blk * 512, R, src)
                sch.barrier()

    def conv_phase(self, li, src):
        sch = self.sch
        S = self.S
        NB = S // 512
        wn = 'm_win%d' % li
        import os
        if os.environ.get('CONV_STOP') == '0':
            return
        with contextlib.ExitStack() as esAB:
            uT = self.sb(esAB, "uT", [128, 8, S + 64], BF16)
            with contextlib.ExitStack() as es:
                T = self.prenorm_tiles(es)
                hT = self.sb(es, "hT", [128, 8, 512], BF16)
                win = self.sb(es, "win", [128, 8, 2048], BF16)
                bin_ = self.sb(es, "bin", [128, 16], F32)
                sg = [self.sb(es, "sg", [128, 512], F32) for _ in range(2)]
                self.wload3(win, wn, ['win'])
                if not os.environ.get('CONV_NOBIN'):
                    sch.dma('sp', bin_[:], self.W('m_bin%d' % li), reads=[self.wk(wn)], writes=['bin'])
                CS = os.environ.get('CONV_STOP', '')
                if CS != 'A1':
                    sch.op('dve', lambda e: e.memset(uT[:, :, 0:30], 0.0), writes=['uT'])
                for blk in range(NB):
                    self.prenorm_block(T, src, blk * 512, 4, li, 0, hT, 'hT')
                    hk = [('hT', c) for c in range(8)]
                    for m in range(8 if CS not in ('A1', 'A2') else 0):
                        ba, bg = (0, 1) if m % 2 == 0 else (2, 3)
                        pa, oa = self.bank(ba)
                        pg, og = self.bank(bg)

                        def mm(e, m=m, pa=pa, oa=oa, pg=pg, og=og):
                            ins = None
                            for c in range(8):
                                ins = e.matmul(pa[:, oa:oa + 512], win[:, c, m * 128:(m + 1) * 128], hT[:, c, :], start=(c == 0), stop=(c == 7))
                            for c in range(8):
                                ins = e.matmul(pg[:, og:og + 512], win[:, c, 1024 + m * 128:1024 + (m + 1) * 128], hT[:, c, :],
                                               start=(c == 0), stop=(c == 7))
                            return ins
                        sch.op('pe', mm, reads=hk + ['win'], writes=[('ps', ba), ('ps', bg)])
                        s_ = sg[m % 2]
                        if CS == 'A3':
                            continue
                        sch.op('act', lambda e, m=m, s_=s_, pg=pg, og=og: e.activation(out=s_[:], in_=pg[:, og:og + 512], func=AF.Sigmoid,
                                                                                     bias=bin_[:, 8 + m:9 + m], scale=1.0),
                               reads=[('ps', bg), 'bin'], writes=[('sg', m % 2)])
                        sch.op('dve', lambda e, m=m, s_=s_, pa=pa, oa=oa, blk=blk: e.scalar_tensor_tensor(
                            out=uT[:, m, 30 + blk * 512: 30 + (blk + 1) * 512], in0=pa[:, oa:oa + 512], scalar=bin_[:, m:m + 1], in1=s_[:],
                            op0=ALU.add, op1=ALU.mult),
                            reads=[('ps', ba), 'bin', ('sg', m % 2)], writes=[('uT', blk)])
                sch.barrier()
            import os
            if os.environ.get('CONV_STOP', '').startswith('A'):
                return
            with contextlib.ExitStack() as es:
                diag = self.sb(es, "diag", [128, 248, 128], BF16)
                dwT = self.sb(es, "dwT", [128, 248], F32)
                dwb = self.sb(es, "dwb", [1, D], BF16)
                bout = self.sb(es, "bout", [1, D], BF16)
                lng = self.sb(es, "lng", [128, 8], F32)
                lnb = self.sb(es, "lnb", [128, 8], F32)
                wout = self.sb(es, "wout", [128, 8, D], BF16)
                wo = 'm_wout%d' % li
                self.wload3(wout, wo, ['wout'])
                sch.dma('sp', dwT[:], self.W('m_dw%d' % li), reads=[self.wk(wo)], writes=['dwT'])
                sch.dma('pool', dwb[:], self.W('m_dwb%d' % li), reads=[self.wk(wo)], writes=['dwb'])
                sch.dma('pool', bout[:], self.W('m_bout%d' % li), reads=[self.wk(wo)], writes=['brow'])
                sch.dma('sp', lng[:], self.W('m_lng%d' % li), reads=[self.wk(wo)], writes=['lng'])
                sch.dma('sp', lnb[:], self.W('m_lnb%d' % li), reads=[self.wk(wo)], writes=['lnb'])
                gB = self.load_grow(es, li, 0)
                R = self.residual_tiles(es, gB)
                vn = self.sb(es, "vn", [128, 4, D], F32)
                sT = self.sb(es, "sT", [128, 8, 512], BF16)
                st = self.sb(es, "cst", [128, 8], F32)
                junk = self.sb(es, "cjunk", [128, D], F32)
                for kq in range(248):
                    sch.op('act', lambda e, kq=kq: e.activation(out=diag[:, kq, :], in_=self.identb[:], func=AF.Identity,
                                                                scale=dwT[:, kq:kq + 1]),
                           reads=['identb', 'dwT'], writes=['diag'])
                for blk in range(NB):
                    for t in range(4):
                        tt = blk * 4 + t
                        pv = self.ps[0]

                        def mmc(e, tt=tt, pv=pv):
                            ins = None
                            for c in range(8):
                                for j in range(31):
                                    ins = e.matmul(pv[:, c * 128:(c + 1) * 128], uT[:, c, tt * 128 + j: tt * 128 + j + 128],
                                                   diag[:, c * 31 + j, :], start=(j == 0), stop=False)
                                ins = e.matmul(pv[:, c * 128:(c + 1) * 128], self.onesb[0:1, 0:128], dwb[0:1, c * 128:(c + 1) * 128],
                                               start=False, stop=True)
                            return ins
                        sch.op('pe', mmc, reads=[('uT', b_) for b_ in range(NB)] + ['uT', 'diag', 'dwb', 'onesb'],
                               writes=[('ps', 0), ('ps', 1)])
                        sch.op('act', lambda e, pv=pv: e.activation(out=junk[:], in_=pv[:, :], func=AF.Identity, accum_out=st[:, 0:1]),
                               reads=[('ps', 0), ('ps', 1)], writes=['cjunk', 'cst'])
                        sch.op('act', lambda e, pv=pv: e.activation(out=junk[:], in_=pv[:, :], func=AF.Square, accum_out=st[:, 1:2]),
                               reads=[('ps', 0), ('ps', 1)], writes=['cjunk', 'cst'])

                        sch.op('dve', lambda e: e.tensor_scalar(out=st[:, 2:3], in0=st[:, 0:1], scalar1=1.0 / D, scalar2=None, op0=ALU.mult),
                               reads=['cst'], writes=['cst'])
                        sch.op('dve', lambda e: e.tensor_tensor(out=st[:, 3:4], in0=st[:, 2:3], in1=st[:, 2:3], op=ALU.mult),
                               reads=['cst'], writes=['cst'])
                        sch.op('dve', lambda e: e.scalar_tensor_tensor(out=st[:, 4:5], in0=st[:, 1:2], scalar=1.0 / D, in1=st[:, 3:4],
                                                                       op0=ALU.mult, op1=ALU.subtract), reads=['cst'], writes=['cst'])
                        self.rstd_ops(st[:, 5:6], st[:, 4:5], 1.0, 'cst', 'cst')
                        sch.op('dve', lambda e: e.scalar_tensor_tensor(out=st[:, 6:7], in0=st[:, 2:3], scalar=-1.0, in1=st[:, 5:6],
                                                                       op0=ALU.mult, op1=ALU.mult), reads=['cst'], writes=['cst'])
                        sch.op('act', lambda e, t=t, pv=pv: e.activation(out=vn[:, t, :], in_=pv[:, :], func=AF.Identity,
                                                                        scale=st[:, 5:6], bias=st[:, 6:7]),
                               reads=[('ps', 0), ('ps', 1), 'cst'], writes=[('vn', t)])
                    for c in range(8):
                        b = 6 + (c % 2)
                        pt, po = self.bank(b)

                        def tr(e, c=c, pt=pt, po=po):
                            ins = None
                            for t in range(4):
                                ins = e.transpose(out=pt[:, po + t * 128: po + (t + 1) * 128], in_=vn[:, t, c * 128:(c + 1) * 128],
                                                  identity=self.ident[:])
                            return ins
                        sch.op('pe', tr, reads=[('vn', t) for t in range(4)] + ['ident'], writes=[('ps', b)])
                        sch.op('act', lambda e, c=c, pt=pt, po=po: e.activation(out=sT[:, c, :], in_=pt[:, po:po + 512], func=AF.Silu,
                                                                               scale=lng[:, c:c + 1], bias=lnb[:, c:c + 1]),
                               reads=[('ps', b), 'lng', 'lnb'], writes=[('sT', c)])
                    self.out_proj2(sT, [('sT', c) for c in range(8)], wout, 4, blk * 512, R, src, bias_row=bout)
                sch.barrier()

    def fox_phase(self, li, src):
        sch = self.sch
        S = self.S
        NB = S // 512
        NTt = S // 128
        qTs = self.dscr("fqTs", [8, 128, S])
        kTs = self.dscr("fkTs", [8, 128, S])
        sogs = self.dscr("sogs", [S, 1024])
        wn = 'm_win%d' % li
        with contextlib.ExitStack() as esAB:
            Vaug = self.sb(esAB, "Vaug", [128, NTt, 16, 65], BF16)
            cum3 = self.sb(esAB, "cum3", [96, S], BF16)
            cposT = self.sb(esAB, "cposT", [128, NTt, 16], F32)
            with contextlib.ExitStack() as es:
                T = self.prenorm_tiles(es)
                hT = self.sb(es, "hT", [128, 8, 512], BF16)
                win = self.sb(es, "win", [128, 8, 4112], BF16)
                bones = self.sb(es, "bones", [128, 128], F32)
                nbf = self.sb(es, "nbf", [16, 1], F32)
                qg = self.sb(es, "qg", [128, 1], F32)
                kg = self.sb(es, "kg", [128, 1], F32)
                sq = [self.sb(es, "sq", [128, 512], F32) for _ in range(2)]
                rs = [self.sb(es, "rs", [128, 512], F32) for _ in range(2)]
                qk = [self.sb(es, "qk", [128, 512], BF16) for _ in range(2)]
                sog = [self.sb(es, "sog", [128, D], BF16) for _ in range(2)]
                cpos = self.sb(es, "cpos", [16, 512], F32)
                carry = self.sb(es, "carry", [16, 1], F32)
                ones16 = self.sb(es, "ones16", [16, 512], F32)
                fe = self.sb(es, "fe", [16, 512], F32)
                fl = fe
                r0 = self.sb(es, "r0", [16, 512], F32)
                hml = [self.sb(es, "hml", [16, 512], BF16) for _ in range(3)]
                self.wload3(win, wn, ['win'])
                sch.dma('sp', bones[:], self.consts[:, C_BONES:C_BONES + 128], writes=['bones'])
                sch.dma('sp', nbf[:], self.W('m_bf%d' % li), reads=[self.wk(wn)], writes=['nbf'])
                sch.dma('sp', qg[:], self.W('m_qg%d' % li), reads=[self.wk(wn)], writes=['qg'])
                sch.dma('sp', kg[:], self.W('m_kg%d' % li), reads=[self.wk(wn)], writes=['kg'])
                sch.op('dve', lambda e: e.tensor_scalar(out=nbf[:], in0=nbf[:], scalar1=-1.0, scalar2=None, op0=ALU.mult), reads=['nbf'], writes=['nbf'])
                sch.op('dve', lambda e: e.tensor_scalar(out=qg[:], in0=qg[:], scalar1=0.125, scalar2=None, op0=ALU.mult), reads=['qg'], writes=['qg'])
                sch.op('dve', lambda e: e.memset(ones16[:], 1.0), writes=['ones16'])
                sch.op('pool', lambda e: e.memset(Vaug[:], 1.0), writes=['Vaug'])
                sch.op('pool', lambda e: e.memset(cum3[:], 0.0), writes=['cum3'])
                mi = 0
                for blk in range(NB):
                    self.prenorm_block(T, src, blk * 512, 4, li, 0, hT, 'hT')
                    hk = [('hT', c) for c in range(8)]
                    for m in range(16):
                        i2 = mi % 2
                        mi += 1
                        b1, b2 = (0, 1) if i2 == 0 else (2, 3)
                        p1, o1 = self.bank(b1)
                        p2, o2 = self.bank(b2)

                        def mm(e, m=m, p1=p1, o1=o1):
                            ins = None
                            for c in range(8):
                                ins = e.matmul(p1[:, o1:o1 + 512], win[:, c, m * 128:(m + 1) * 128], hT[:, c, :], start=(c == 0), stop=(c == 7))
                            return ins
                        sch.op('pe', mm, reads=hk + ['win'], writes=[('ps', b1)])
                        sch.op('act', lambda e, i2=i2, p1=p1, o1=o1: e.activation(out=sq[i2][:], in_=p1[:, o1:o1 + 512], func=AF.Square),
                               reads=[('ps', b1)], writes=[('sq', i2)])
                        sch.op('pe', lambda e, i2=i2, p2=p2, o2=o2: e.matmul(p2[:, o2:o2 + 512], bones[:, :], sq[i2][:], start=True, stop=True),
                               reads=['bones', ('sq', i2)], writes=[('ps', b2)])
                        self.rstd_ops(rs[i2][:], p2[:, o2:o2 + 512], 1.0 / 64.0, ('ps', b2), ('rs', i2))
                        g_ = qg if m < 8 else kg
                        sch.op('dve', lambda e, i2=i2, p1=p1, o1=o1, g_=g_: e.scalar_tensor_tensor(
                            out=qk[i2][:], in0=p1[:, o1:o1 + 512], scalar=g_[:, 0:1], in1=rs[i2][:], op0=ALU.mult, op1=ALU.mult),
                            reads=[('ps', b1), ('rs', i2), 'qg', 'kg'], writes=[('qk', i2)])
                        dst = qTs if m < 8 else kTs
                        sch.dma('sp', dst[m % 8, :, blk * 512:(blk + 1) * 512], qk[i2][:], reads=[('qk', i2)], writes=[('qkTs', blk)])
                    for t in range(4):
                        tt = blk * 4 + t
                        pv = self.ps[2]

                        def mmv(e, t=t, base=2048, pv=pv):
                            ins = None
                            for n in range(2):
                                for c in range(8):
                                    ins = e.matmul(pv[:, n * 512:(n + 1) * 512], hT[:, c, t * 128:(t + 1) * 128],
                                                   win[:, c, base + n * 512: base + (n + 1) * 512], start=(c == 0), stop=(c == 7))
                            return ins
                        sch.op('pe', mmv, reads=hk + ['win'], writes=[('ps', 4), ('ps', 5)])
                        sch.op('act', lambda e, tt=tt, pv=pv: e.activation(out=Vaug[:, tt, :, 0:64], in_=pv[:, :].rearrange("p (h d) -> p h d", h=16),
                                                                          func=AF.Identity),
                               reads=[('ps', 4), ('ps', 5)], writes=['Vaug'])
                        pr = self.ps[3]

                        def mmo(e, t=t, base=3088, pr=pr):
                            ins = None
                            for n in range(2):
                                for c in range(8):
                                    ins = e.matmul(pr[:, n * 512:(n + 1) * 512], hT[:, c, t * 128:(t + 1) * 128],
                                                   win[:, c, base + n * 512: base + (n + 1) * 512], start=(c == 0), stop=(c == 7))
                            return ins
                        sch.op('pe', mmo, reads=hk + ['win'], writes=[('ps', 6), ('ps', 7)])
                        sch.op('act', lambda e, t=t, pr=pr: e.activation(out=sog[t % 2][:], in_=pr[:, :], func=AF.Sigmoid),
                               reads=[('ps', 6), ('ps', 7)], writes=[('sog', t % 2)])
                        sch.dma('sp', sogs[tt * 128:(tt + 1) * 128, :], sog[t % 2][:], reads=[('sog', t % 2)], writes=[('sogs', tt)])
                    p3, o3 = self.bank(3)

                    def mmf(e, p3=p3, o3=o3):
                        ins = None
                        for c in range(8):
                            ins = e.matmul(p3[0:16, o3:o3 + 512], win[:, c, 3072:3088], hT[:, c, :], start=(c == 0), stop=(c == 7))
                        return ins
                    sch.op('pe', mmf, reads=hk + ['win'], writes=[('ps', 3)])
                    sch.op('act', lambda e, p3=p3, o3=o3: e.activation(out=fe[:], in_=p3[0:16, o3:o3 + 512], func=AF.Exp, scale=-1.0, bias=nbf[0:16, :]),
                           reads=[('ps', 3), 'nbf'], writes=['fe'])
                    sch.op('act', lambda e: e.activation(out=fl[:], in_=fe[:], func=AF.Ln, bias=self.onec[0:16, :], scale=1.0),
                           reads=['fe', 'onec'], writes=['fe'])
                    init = 0.0 if blk == 0 else carry[:, 0:1]
                    sch.op('dve', lambda e, blk=blk, init=init: e.tensor_tensor_scan(
                        out=cpos[:, :], data0=ones16[:], data1=fl[:], initial=init, op0=ALU.mult, op1=ALU.add),
                        reads=['fe', 'ones16', 'carry'], writes=['cpos'])
                    sch.op('dve', lambda e: e.tensor_copy(out=carry[:, 0:1], in_=cpos[:, 511:512]), reads=['cpos'], writes=['carry'])

                    sch.op('dve', lambda e, blk=blk: e.tensor_scalar(out=r0[:], in0=cpos[:, :], scalar1=-1.0, scalar2=None,
                                                                    op0=ALU.mult), reads=['cpos'], writes=['r0'])
                    for j in range(3):
                        sch.op('dve', lambda e, j=j: e.tensor_copy(out=hml[j][:], in_=r0[:]), reads=['r0'], writes=[('hml', j)])
                        if j < 2:
                            sch.op('dve', lambda e, j=j: e.tensor_tensor(out=r0[:], in0=r0[:], in1=hml[j][:], op=ALU.subtract),
                                   reads=['r0', ('hml', j)], writes=['r0'])
                    for j in range(3):
                        sch.dma('sp', cum3[32 * j:32 * j + 16, blk * 512:(blk + 1) * 512], hml[j][:], reads=[('hml', j)], writes=['cum3'])
                    for t in range(4):
                        tt = blk * 4 + t
                        p0, o0 = self.bank(t % 2)
                        sch.op('pe', lambda e, t=t, p0=p0, o0=o0: e.transpose(out=p0[:, o0:o0 + 16], in_=cpos[0:16, t * 128:(t + 1) * 128],
                                                                              identity=self.ident[0:16, 0:16]),
                               reads=['cpos', 'ident'], writes=[('ps', t % 2)])
                        sch.op('dve', lambda e, tt=tt, p0=p0, o0=o0: e.tensor_copy(out=cposT[:, tt, :], in_=p0[:, o0:o0 + 16]),
                               reads=[('ps', t % 2)], writes=['cposT'])
                sch.barrier()
            import os
            if os.environ.get('FOX_STOP') == 'A':
                return
            with contextlib.ExitStack() as es:
                sel = self.sb(es, "sel", [96, 16, 128], BF16)
                maskb = self.sb(es, "maskb", [128, 128], BF16)
                wout = self.sb(es, "wout", [128, 8, D], BF16)
                wo = 'm_wout%d' % li
                self.wload3(wout, wo, ['wout'])
                sch.dma('pool', sel[:], self.consts[0:96, C_SEL:C_SEL + 2048].rearrange("p (h m) -> p h m", h=16), writes=['sel'])
                sch.dma('pool', maskb[:], self.consts[:, C_MASK:C_MASK + 128], writes=['maskb'])
                gB = self.load_grow(es, li, 0)
                R = self.residual_tiles(es, gB)
                qT = [self.sb(es, "fqT", [128, 8, 512], BF16) for _ in range(2)]
                kT = [self.sb(es, "fkT", [128, S], BF16) for _ in range(2)]
                sg = self.sb(es, "fsog", [128, 4, D], BF16)
                O = self.sb(es, "fO", [128, 4, D], F32)
                PT = [self.sb(es, "PT", [128, 512], BF16) for _ in range(3)]
                rc = self.sb(es, "rc", [128, 4], F32)
                OT = self.sb(es, "fOT", [128, 8, 512], BF16)
                ki = 0
                pi = 0
                for qb in range(NB):
                    i = qb % 2
                    nk = (qb + 1) * 512
                    sch.dma('sp', qT[i][:], qTs[:, :, qb * 512:(qb + 1) * 512].rearrange("m p s -> p m s"), writes=[('fqT', i)])
                    sch.dma('sp', sg[:], sogs[qb * 512:(qb + 1) * 512, :].rearrange("(t p) f -> p t f", p=128), writes=['fsog'])
                    for m in range(8):
                        j = ki % 2
                        ki += 1
                        sch.dma('sp', kT[j][:, 0:nk], kTs[m, :, 0:nk], writes=[('fkT', j)])
                        for hh in range(2):
                            h = 2 * m + hh
                            pb = hh * 64
                            ab = 3 if h % 2 == 0 else 7
                            pa, oa = self.bank(ab)
                            nkt = 4 * qb + 4
                            for kt in range(nkt):
                                q0 = max(0, kt - 4 * qb)
                                qc0 = q0 * 128
                                sb_ = kt % 3
                                pst, ost = self.bank(sb_)
                                k3 = pi % 3
                                pi += 1
                                diagt = kt >= 4 * qb

                                def mms(e, i=i, j=j, m=m, h=h, pb=pb, kt=kt, qc0=qc0, pst=pst, ost=ost, diagt=diagt, qb=qb):
                                    lk = kT[j][pb:pb + 64, kt * 128:(kt + 1) * 128]
                                    if diagt:
                                        e.matmul(pst[:, ost + qc0:ost + qc0 + 128], lk, qT[i][pb:pb + 64, m, qc0:qc0 + 128], start=True, stop=False)
                                        e.matmul(pst[:, ost + qc0:ost + qc0 + 128], sel[0:96, h, :], cum3[0:96, qb * 512 + qc0: qb * 512 + qc0 + 128],
                                                 start=False, stop=False)
                                        ins = e.matmul(pst[:, ost + qc0:ost + qc0 + 128], self.identb[:, :], maskb[:, :], start=False, stop=True)
                                        lo = qc0 + 128
                                    else:
                                        lo = qc0
                                        ins = None
                                    if lo < 512:
                                        e.matmul(pst[:, ost + lo:ost + 512], lk, qT[i][pb:pb + 64, m, lo:512], start=True, stop=False)
                                        ins = e.matmul(pst[:, ost + lo:ost + 512], sel[0:96, h, :], cum3[0:96, qb * 512 + lo:(qb + 1) * 512],
                                                       start=False, stop=True)
                                    return ins
                                sch.op('pe', mms, reads=[('fkT', j), ('fqT', i), 'sel', 'cum3', 'identb', 'maskb'], writes=[('ps', sb_)])
                                sch.op('act', lambda e, k3=k3, kt=kt, h=h, qc0=qc0, pst=pst, ost=ost: e.activation(
                                    out=PT[k3][:, qc0:512], in_=pst[:, ost + qc0:ost + 512], func=AF.Exp, bias=cposT[:, kt, h:h + 1], scale=1.0),
                                    reads=[('ps', sb_), 'cposT'], writes=[('PT', k3)])

                                def mmpv(e, k3=k3, kt=kt, h=h, q0=q0, qb=qb, pa=pa, oa=oa):
                                    ins = None
                                    for qs in range(q0, 4):
                                        ins = e.matmul(pa[:, oa + qs * 65: oa + qs * 65 + 65], PT[k3][:, qs * 128:(qs + 1) * 128],
                                                       Vaug[:, kt, h, :], start=(kt == 0), stop=(kt == 4 * qb + qs))
                                    return ins
                                sch.op('pe', mmpv, reads=[('PT', k3), 'Vaug'], writes=[('ps', ab)])

                            sch.op('dve', lambda e, pa=pa, oa=oa: e.reciprocal(
                                out=rc[:, 0:4], in_=pa[:, oa:oa + 260].rearrange("p (q d) -> p q d", q=4)[:, :, 64]),
                                reads=[('ps', ab)], writes=['rc'])

                            def norm(e, h=h, pa=pa, oa=oa):
                                ins = None
                                for qs in range(4):
                                    ins = e.tensor_scalar(out=O[:, qs, h * 64:(h + 1) * 64], in0=pa[:, oa + qs * 65: oa + qs * 65 + 64],
                                                          scalar1=rc[:, qs:qs + 1], scalar2=None, op0=ALU.mult)
                                return ins
                            sch.op('dve', norm, reads=[('ps', ab), 'rc'], writes=[('fO', h)] + [('fOg', t_) for t_ in range(4)])
                    for t in range(4):
                        sch.op('pool', lambda e, t=t: e.tensor_tensor(out=O[:, t, :], in0=O[:, t, :], in1=sg[:, t, :], op=ALU.mult),
                               reads=[('fO', h_) for h_ in range(16)] + ['fsog'], writes=[('fOg', t)])
                    for c in range(8):
                        b = 6 if c % 2 == 0 else 2
                        pt, po = self.bank(b)

                        def tr(e, c=c, pt=pt, po=po):
                            ins = None
                            for t in range(4):
                                ins = e.transpose(out=pt[:, po + t * 128: po + (t + 1) * 128], in_=O[:, t, c * 128:(c + 1) * 128],
                                                  identity=self.ident[:])
                            return ins
                        sch.op('pe', tr, reads=[('fOg', t) for t in range(4)] + ['ident'], writes=[('ps', b)])
                        sch.op('act', lambda e, c=c, pt=pt, po=po: e.activation(out=OT[:, c, :], in_=pt[:, po:po + 512], func=AF.Identity),
                               reads=[('ps', b)], writes=[('fOT', c)])
                    self.out_proj2(OT, [('fOT', c) for c in range(8)], wout, 4, qb * 512, R, src)
                sch.barrier()

_CACHE = {}


def run_cfg(cfg, inputs):
    key = repr(sorted(cfg.items(), key=lambda kv: kv[0]))
    if key not in _CACHE:
        _CACHE[key] = Prog(cfg).build()
    nc = _CACHE[key]
    n = cfg.get('NCORE', NCORES)
    nseq = cfg.get('NSEQ', 1)
    coll = cfg.get('COLL', True)
    S = cfg['S']
    packed = pack_weights(cfg, inputs)
    consts = make_consts()
    x = np.asarray(inputs['x'], dtype=np.float32)
    c = np.asarray(inputs['c'], dtype=np.float32)
    assert n * nseq == x.shape[0]
    if not coll:
        full = [np.ascontiguousarray(pk.reshape(-1, WC)) for pk in packed]
    in_maps = []
    for b in range(n):
        m = {"x": np.ascontiguousarray(x[b * nseq:(b + 1) * nseq].reshape(nseq * S, D)),
             "cvec": np.ascontiguousarray(np.concatenate([colform(c[b * nseq + q]) for q in range(nseq)], axis=1)),
             "consts": consts}
        for g, pk in enumerate(packed):
            m["wsh%d" % g] = pk[b] if coll else full[g]
        in_maps.append(m)
    res = run_bass_kernel_spmd(nc, in_maps, core_ids=list(range(n)))
    return np.concatenate([res.results[b]["y"].reshape(nseq, S, D) for b in range(n)], axis=0)


FULL_CFG = dict(S=4096, layers=[('gla', 'ffn'), ('conv', 'moe'), ('fox', 'ffn'), ('gla', 'moe')],
                DFF=2816, DFFE=3584, E=8, NCORE=8, NSEQ=1, COLL=False)


def kernel(**inputs):
    return run_cfg(FULL_CFG, inputs).astype(np.float32)
```
